# Optimizing a Trainium2 kernel written in Bass

```python
import jax
import jax.numpy as jnp
from jax import lax

D_MODEL = 1024
BATCH = 8
SEQ = 4096
DEPTH = 4

MEM_LEN = 256
N_MIXERS = 3
HEAD_DIM = 64
N_MIX_HEADS = 12
MIX_WIDTH = N_MIX_HEADS * HEAD_DIM
N_MEM_HEADS = 4
MEM_WIDTH = N_MEM_HEADS * HEAD_DIM
OUT_WIDTH = MIX_WIDTH + MEM_WIDTH
Q_BLOCK = 128
MLA_Q_RANK = 256
MLA_KV_RANK = 128
MLA_NOPE_DIM = 64
MLA_ROPE_DIM = 32
MLA_QK_DIM = MLA_NOPE_DIM + MLA_ROPE_DIM
MLA_V_DIM = 64
ROPE_THETA = 10000.0
MOBA_BLOCK = 256
MOBA_TOPK = 3
D_FF = 2816
CONV_WIDTH = 3
EPS = 1e-6
POS_OFFSET_MAX = 1024

FOX_IN = 3 * MIX_WIDTH + N_MIX_HEADS + MEM_WIDTH
MLA_IN = MLA_Q_RANK + MLA_KV_RANK + MLA_ROPE_DIM + MEM_WIDTH
MOBA_IN = 3 * MIX_WIDTH + MEM_WIDTH

kernel_name = 'hybrid_fox_mla_moba_memxattn_convffn'


def rmsnorm(x, g):
    xf = x.astype(jnp.float32)
    y = xf * lax.rsqrt(jnp.mean(xf * xf, axis=-1, keepdims=True) + EPS)
    return (y * g.astype(jnp.float32)).astype(x.dtype)


def alibi_slopes(n_heads):
    return 2.0 ** (-8.0 * jnp.arange(1, n_heads + 1, dtype=jnp.float32) / n_heads)


def rope(x, cos, sin):
    half = x.shape[-1] // 2
    x1, x2 = x[..., :half], x[..., half:]
    return jnp.concatenate([x1 * cos - x2 * sin, x1 * sin + x2 * cos], axis=-1)


def to_heads(t, n_heads):
    b, s, _ = t.shape
    return t.reshape(b, s, n_heads, -1).transpose(0, 2, 1, 3)


def from_heads(t):
    b, h, s, d = t.shape
    return t.transpose(0, 2, 1, 3).reshape(b, s, h * d)


def causal_block_attention(q, k, v, log_decay=None):
    b, h, s, dk = q.shape
    nq = s // Q_BLOCK
    q_chunks = q.reshape(b, h, nq, Q_BLOCK, dk).transpose(2, 0, 1, 3, 4)
    key_idx = jnp.arange(s)
    xs = [q_chunks, jnp.arange(nq)]
    if log_decay is not None:
        xs.append(log_decay.reshape(b, h, nq, Q_BLOCK).transpose(2, 0, 1, 3))

    def step(args):
        qc, i = args[0], args[1]
        logits = jnp.einsum('bhqd,bhkd->bhqk', qc, k).astype(jnp.float32)
        if log_decay is not None:
            logits = logits + args[2][..., None] - log_decay[:, :, None, :]
        q_idx = i * Q_BLOCK + jnp.arange(Q_BLOCK)
        logits = jnp.where(key_idx[None, :] <= q_idx[:, None], logits, -jnp.inf)
        p = jax.nn.softmax(logits, axis=-1).astype(v.dtype)
        return jnp.einsum('bhqk,bhkd->bhqd', p, v)

    out = lax.map(step, tuple(xs))
    return out.transpose(1, 2, 0, 3, 4).reshape(b, h, s, v.shape[-1])


def fox_mixer(p, b_f, q_gain, k_gain):
    q, k, v, f = jnp.split(p, [MIX_WIDTH, 2 * MIX_WIDTH, 3 * MIX_WIDTH], axis=-1)
    q = rmsnorm(to_heads(q, N_MIX_HEADS), q_gain) * HEAD_DIM ** -0.5
    k = rmsnorm(to_heads(k, N_MIX_HEADS), k_gain)
    log_f = jax.nn.log_sigmoid(f.astype(jnp.float32) + b_f.astype(jnp.float32))
    cum = lax.cumsum(log_f, axis=1).transpose(0, 2, 1)
    return from_heads(causal_block_attention(q, k, to_heads(v, N_MIX_HEADS), cum))


def mla_mixer(p, cos, sin, qa_norm, kva_norm, w_q_up, w_kv_up, q_gain, k_gain):
    b, s, _ = p.shape
    q_lat, kv_lat, k_r = jnp.split(p, [MLA_Q_RANK, MLA_Q_RANK + MLA_KV_RANK], axis=-1)
    q = (rmsnorm(q_lat, qa_norm) @ w_q_up).reshape(b, s, N_MIX_HEADS, MLA_QK_DIM)
    kv = (rmsnorm(kv_lat, kva_norm) @ w_kv_up).reshape(b, s, N_MIX_HEADS, MLA_NOPE_DIM + MLA_V_DIM)
    q = jnp.concatenate([q[..., :MLA_NOPE_DIM], rope(q[..., MLA_NOPE_DIM:], cos, sin)], axis=-1)
    k_rope = jnp.broadcast_to(rope(k_r[:, :, None, :], cos, sin), (b, s, N_MIX_HEADS, MLA_ROPE_DIM))
    k = jnp.concatenate([kv[..., :MLA_NOPE_DIM], k_rope], axis=-1)
    v = kv[..., MLA_NOPE_DIM:]
    q = rmsnorm(q, q_gain).transpose(0, 2, 1, 3) * MLA_QK_DIM ** -0.5
    k = rmsnorm(k, k_gain).transpose(0, 2, 1, 3)
    return from_heads(causal_block_attention(q, k, v.transpose(0, 2, 1, 3)))


def moba_attention(q, k, v, positions, slopes):
    b, h, s, d = q.shape
    nb = -(-s // MOBA_BLOCK)
    pad = nb * MOBA_BLOCK - s
    kb = jnp.pad(k, ((0, 0), (0, 0), (0, pad), (0, 0))).reshape(b, h, nb, MOBA_BLOCK, d)
    vb = jnp.pad(v, ((0, 0), (0, 0), (0, pad), (0, 0))).reshape(b, h, nb, MOBA_BLOCK, d)
    posb = jnp.pad(positions, ((0, 0), (0, pad))).reshape(b, nb, MOBA_BLOCK)
    n_sel = min(MOBA_TOPK, nb - 1)
    nq = s // Q_BLOCK

    def chunks(t):
        tail = t.shape[3:]
        t = t.reshape((b, h, nq, Q_BLOCK) + tail)
        perm = (0, 2, 1, 3) + tuple(range(4, t.ndim))
        return t.transpose(perm).reshape((b * nq, h, Q_BLOCK) + tail)

    xs = [chunks(q), positions.reshape(b * nq, Q_BLOCK),
          jnp.repeat(jnp.arange(b), nq), jnp.tile(jnp.arange(nq), b)]
    if n_sel > 0:
        k_mean = kb.mean(axis=3)
        gate = jnp.einsum('bhsd,bhnd->bhsn', q, k_mean).astype(jnp.float32)
        q_blk = jnp.arange(s) // MOBA_BLOCK
        gate = jnp.where(jnp.arange(nb)[None, :] < q_blk[:, None], gate, -jnp.inf)
        g_val, g_idx = lax.top_k(gate, n_sel)
        xs += [chunks(g_idx), chunks(jnp.isfinite(g_val))]

    def step(args):
        qc, pq, bi, ci = args[0], args[1], args[2], args[3]
        t_idx = ci * Q_BLOCK + jnp.arange(Q_BLOCK)
        own = (ci * Q_BLOCK) // MOBA_BLOCK
        kb_b, vb_b, posb_b = kb[bi], vb[bi], posb[bi]
        k_own = lax.dynamic_index_in_dim(kb_b, own, axis=1, keepdims=False)
        v_own = lax.dynamic_index_in_dim(vb_b, own, axis=1, keepdims=False)
        pos_own = lax.dynamic_index_in_dim(posb_b, own, axis=0, keepdims=False)
        dist_own = (pq[:, None] - pos_own[None, :]).astype(jnp.float32)
        s_own = (jnp.einsum('hqd,hjd->hqj', qc, k_own).astype(jnp.float32)
                 - slopes[:, None, None] * dist_own[None])
        own_idx = own * MOBA_BLOCK + jnp.arange(MOBA_BLOCK)
        s_own = jnp.where(own_idx[None, None, :] <= t_idx[None, :, None], s_own, -jnp.inf)
        if n_sel == 0:
            p_own = jax.nn.softmax(s_own, axis=-1).astype(v.dtype)
            return jnp.einsum('hqj,hjd->hqd', p_own, v_own)
        idx, ok = args[4], args[5]
        k_sel = jax.vmap(lambda blocks, i: blocks[i])(kb_b, idx)
        v_sel = jax.vmap(lambda blocks, i: blocks[i])(vb_b, idx)
        dist_sel = (pq[None, :, None, None] - posb_b[idx]).astype(jnp.float32)
        s_sel = (jnp.einsum('hqd,hqkjd->hqkj', qc, k_sel).astype(jnp.float32)
                 - slopes[:, None, None, None] * dist_sel)
        s_sel = jnp.where(ok[..., None], s_sel, -jnp.inf).reshape(h, Q_BLOCK, n_sel * MOBA_BLOCK)
        p = jax.nn.softmax(jnp.concatenate([s_sel, s_own], axis=-1), axis=-1).astype(v.dtype)
        p_sel = p[..., :n_sel * MOBA_BLOCK].reshape(h, Q_BLOCK, n_sel, MOBA_BLOCK)
        p_own = p[..., n_sel * MOBA_BLOCK:]
        return (jnp.einsum('hqkj,hqkjd->hqd', p_sel, v_sel)
                + jnp.einsum('hqj,hjd->hqd', p_own, v_own))

    out = lax.map(step, tuple(xs))
    return out.reshape(b, nq, h, Q_BLOCK, d).transpose(0, 2, 1, 3, 4).reshape(b, h, s, d)


def moba_mixer(p, positions, q_gain, k_gain):
    q, k, v = jnp.split(p, [MIX_WIDTH, 2 * MIX_WIDTH], axis=-1)
    q = rmsnorm(to_heads(q, N_MIX_HEADS), q_gain) * HEAD_DIM ** -0.5
    k = rmsnorm(to_heads(k, N_MIX_HEADS), k_gain)
    out = moba_attention(q, k, to_heads(v, N_MIX_HEADS), positions, alibi_slopes(N_MIX_HEADS))
    return from_heads(out)


def memory_cross(qm, k_mem, v_mem, q_gain, k_gain):
    b, s, _ = qm.shape
    q = rmsnorm(qm.reshape(b, s, N_MEM_HEADS, HEAD_DIM), q_gain) * HEAD_DIM ** -0.5
    k = rmsnorm(k_mem, k_gain)
    logits = jnp.einsum('bshd,bmhd->bhsm', q, k).astype(jnp.float32)
    p = jax.nn.softmax(logits, axis=-1).astype(v_mem.dtype)
    return jnp.einsum('bhsm,bmhd->bshd', p, v_mem).reshape(b, s, MEM_WIDTH)


def conv_ffn(h, w_up, conv_w, conv_b, w_down):
    u = h @ w_up
    c = u.shape[-1]
    u = lax.conv_general_dilated(u, conv_w[:, None, :].astype(u.dtype), window_strides=(1,),
                                 padding=[(CONV_WIDTH - 1, 0)],
                                 dimension_numbers=('NWC', 'WIO', 'NWC'),
                                 feature_group_count=c) + conv_b
    gate, val = jnp.split(u, 2, axis=-1)
    return (jax.nn.silu(gate) * val) @ w_down


def setup_inputs(seed: int = 0) -> dict:
    key = jax.random.key(seed)
    ks = iter(jax.random.split(key, 32))
    f32 = jnp.float32

    def dense(shape):
        return jax.random.normal(next(ks), shape, f32) * shape[-2] ** -0.5

    def gain(shape):
        return 1.0 + 0.05 * jax.random.normal(next(ks), shape, f32)

    n_fox = len(range(0, DEPTH, N_MIXERS))
    n_mla = len(range(1, DEPTH, N_MIXERS))
    n_moba = len(range(2, DEPTH, N_MIXERS))
    x = jax.random.normal(next(ks), (BATCH, SEQ, D_MODEL), f32)
    mem = jax.random.normal(next(ks), (BATCH, MEM_LEN, D_MODEL), f32)
    positions = (jnp.arange(SEQ, dtype=jnp.int32)[None, :]
                 + jax.random.randint(next(ks), (BATCH, 1), 0, POS_OFFSET_MAX, dtype=jnp.int32))
    return {
        'x': x,
        'mem': mem,
        'positions': positions,
        'norm_mix': gain((DEPTH, D_MODEL)),
        'norm_ffn': gain((DEPTH, D_MODEL)),
        'w_out': dense((DEPTH, OUT_WIDTH, D_MODEL)),
        'mem_norm': gain((D_MODEL,)),
        'w_mem_kv': dense((D_MODEL, 2 * MEM_WIDTH)),
        'mem_q_gain': gain((DEPTH, HEAD_DIM)),
        'mem_k_gain': gain((DEPTH, HEAD_DIM)),
        'fox_w_in': dense((n_fox, D_MODEL, FOX_IN)),
        'fox_b_f': jax.random.uniform(next(ks), (n_fox, N_MIX_HEADS), f32, 1.0, 5.0),
        'fox_q_gain': gain((n_fox, HEAD_DIM)),
        'fox_k_gain': gain((n_fox, HEAD_DIM)),
        'mla_w_in': dense((n_mla, D_MODEL, MLA_IN)),
        'mla_qa_norm': gain((n_mla, MLA_Q_RANK)),
        'mla_kva_norm': gain((n_mla, MLA_KV_RANK)),
        'mla_w_q_up': dense((n_mla, MLA_Q_RANK, N_MIX_HEADS * MLA_QK_DIM)),
        'mla_w_kv_up': dense((n_mla, MLA_KV_RANK, N_MIX_HEADS * (MLA_NOPE_DIM + MLA_V_DIM))),
        'mla_q_gain': gain((n_mla, MLA_QK_DIM)),
        'mla_k_gain': gain((n_mla, MLA_QK_DIM)),
        'moba_w_in': dense((n_moba, D_MODEL, MOBA_IN)),
        'moba_q_gain': gain((n_moba, HEAD_DIM)),
        'moba_k_gain': gain((n_moba, HEAD_DIM)),
        'ffn_w_up': dense((DEPTH, D_MODEL, 2 * D_FF)),
        'ffn_conv_w': dense((DEPTH, CONV_WIDTH, 2 * D_FF)),
        'ffn_conv_b': 0.01 * jax.random.normal(next(ks), (DEPTH, 2 * D_FF), f32),
        'ffn_w_down': dense((DEPTH, D_FF, D_MODEL)),
    }


def reference(x, mem, positions, norm_mix, norm_ffn, w_out, mem_norm, w_mem_kv, mem_q_gain,
              mem_k_gain, fox_w_in, fox_b_f, fox_q_gain, fox_k_gain, mla_w_in, mla_qa_norm,
              mla_kva_norm, mla_w_q_up, mla_w_kv_up, mla_q_gain, mla_k_gain, moba_w_in,
              moba_q_gain, moba_k_gain, ffn_w_up, ffn_conv_w, ffn_conv_b, ffn_w_down):
    b, m = mem.shape[0], mem.shape[1]
    mem_kv = rmsnorm(mem, mem_norm) @ w_mem_kv
    k_mem = mem_kv[..., :MEM_WIDTH].reshape(b, m, N_MEM_HEADS, HEAD_DIM)
    v_mem = mem_kv[..., MEM_WIDTH:].reshape(b, m, N_MEM_HEADS, HEAD_DIM)
    inv_freq = ROPE_THETA ** (-jnp.arange(0, MLA_ROPE_DIM, 2, dtype=jnp.float32) / MLA_ROPE_DIM)
    ang = positions.astype(jnp.float32)[..., None] * inv_freq
    cos = jnp.cos(ang)[:, :, None, :].astype(x.dtype)
    sin = jnp.sin(ang)[:, :, None, :].astype(x.dtype)

    for i in range(DEPTH):
        kind, j = i % N_MIXERS, i // N_MIXERS
        h = rmsnorm(x, norm_mix[i])
        if kind == 0:
            p = h @ fox_w_in[j]
            o_mix = fox_mixer(p[..., :-MEM_WIDTH], fox_b_f[j], fox_q_gain[j], fox_k_gain[j])
        elif kind == 1:
            p = h @ mla_w_in[j]
            o_mix = mla_mixer(p[..., :-MEM_WIDTH], cos, sin, mla_qa_norm[j], mla_kva_norm[j],
                              mla_w_q_up[j], mla_w_kv_up[j], mla_q_gain[j], mla_k_gain[j])
        else:
            p = h @ moba_w_in[j]
            o_mix = moba_mixer(p[..., :-MEM_WIDTH], positions, moba_q_gain[j], moba_k_gain[j])
        o_mem = memory_cross(p[..., -MEM_WIDTH:], k_mem, v_mem, mem_q_gain[i], mem_k_gain[i])
        x = x + jnp.concatenate([o_mix, o_mem], axis=-1) @ w_out[i]
        x = x + conv_ffn(rmsnorm(x, norm_ffn[i]), ffn_w_up[i], ffn_conv_w[i], ffn_conv_b[i],
                         ffn_w_down[i])
    return x
```

```python
import math
import numpy as np
import concourse.bass as bass
import concourse.mybir as mybir
from concourse.bass_utils import run_bass_kernel_spmd

F32 = mybir.dt.float32
BF16 = mybir.dt.bfloat16
I32 = mybir.dt.int32
ALU = mybir.AluOpType
AF = mybir.ActivationFunctionType
AX = mybir.AxisListType


class Buf:
    __slots__ = ("name", "w", "r")

    def __init__(self, name):
        self.name = name
        self.w = None
        self.r = {}


class Prog:
    CE = ("act", "pool", "pe", "dve")

    def __init__(self, nc, nslots=None):
        self.nc = nc
        nslots = nslots or {"sp": 12, "act": 4, "pool": 8}
        self.streams = {e: [] for e in ("sp", "act", "pool", "pe", "dve")}
        self.slots = {q: [0] * n for q, n in nslots.items()}
        self.slot_ptr = {q: 0 for q in nslots}
        self.sb_off = 0
        self.sb_hi = 0
        self.n_alloc = 0
        self.sb_words = 52992
        self.big = nc.alloc_sbuf_tensor("bigsb", [128, self.sb_words], F32)

    def sbuf(self, name, shape, dtype, align=64):
        esz = {F32: 4, BF16: 2, I32: 4}[dtype]
        nel = int(np.prod(shape[1:]))
        nbytes = (nel * esz + 3) // 4 * 4
        off = (self.sb_off + align - 1) // align * align
        assert off + nbytes <= self.sb_words * 4, f"SBUF overflow allocating {name}: {off + nbytes}"
        self.sb_off = off + nbytes
        self.sb_hi = max(self.sb_hi, self.sb_off)
        v = self.big[0:shape[0], off // 4: off // 4 + nbytes // 4]
        if dtype != F32:
            v = v.bitcast(dtype)
            v = v[:, 0:nel]
        if len(shape) == 3:
            v = v.rearrange("p (a b) -> p a b", b=shape[2])
        elif len(shape) == 4:
            v = v.rearrange("p (a b c) -> p a b c", b=shape[2], c=shape[3])
        return v

    def mark(self):
        return self.sb_off

    def release(self, m):
        self.sb_off = m

    def _deps(self, eng, reads, writes):
        toks = []
        for b in reads:
            if b.w is not None:
                toks.append(b.w)
        for b in writes:
            if b.w is not None:
                toks.append(b.w)
            toks.extend(b.r.values())
        out = []
        for t in toks:
            if t[0] == "c":
                if t[1] == eng and eng == "pe":
                    continue
                self.streams[t[1]][t[2]]["sig"] = True
            out.append(t)
        return out

    def _commit(self, tok, reads, writes):
        key = tok[1]
        for b in writes:
            b.w = tok
            b.r = {}
        for b in reads:
            if b in writes:
                continue
            b.r[key] = tok

    def op(self, eng, fn, reads=(), writes=()):
        st = self.streams[eng]
        idx = len(st)
        waits = self._deps(eng, reads, writes)
        st.append(dict(fn=fn, waits=waits, sig=False, dma=None))
        tok = ("c", eng, idx)
        self._commit(tok, reads, writes)
        return tok

    def dma(self, q, out, in_, reads=(), writes=(), **kw):
        st = self.streams[q]
        waits = self._deps(q, reads, writes)
        n = len(self.slots[q])
        s = self.slot_ptr[q]
        self.slot_ptr[q] = (s + 1) % n
        prev = self.slots[q][s]
        if prev > 0:
            waits.append(("d", (q, s), prev))
        self.slots[q][s] = prev + 16
        tok = ("d", (q, s), prev + 16)
        st.append(dict(fn=lambda e: e.dma_start(out=out, in_=in_, **kw), waits=waits, sig=False, dma=(q, s)))
        self._commit(tok, reads, writes)
        return tok


    def act(self, out, in_, func, reads=(), writes=(), **kw):
        return self.op("act", lambda e: e.activation(out=out, in_=in_, func=func, **kw), reads, writes)

    def tt(self, eng, out, in0, in1, op, reads=(), writes=()):
        return self.op(eng, lambda e: e.tensor_tensor(out=out, in0=in0, in1=in1, op=op), reads, writes)

    def ts(self, eng, out, in0, s1, s2, op0, op1=None, reads=(), writes=()):
        if op1 is None:
            return self.op(eng, lambda e: e.tensor_scalar(out=out, in0=in0, scalar1=s1, scalar2=None, op0=op0), reads, writes)
        return self.op(eng, lambda e: e.tensor_scalar(out=out, in0=in0, scalar1=s1, scalar2=s2, op0=op0, op1=op1), reads, writes)

    def stt(self, out, in0, scalar, in1, op0, op1, reads=(), writes=()):
        return self.op("dve", lambda e: e.scalar_tensor_tensor(out=out, in0=in0, scalar=scalar, in1=in1, op0=op0, op1=op1), reads, writes)

    def copy(self, eng, out, in_, reads=(), writes=()):
        if eng == "act":
            return self.op("act", lambda e: e.copy(out=out, in_=in_), reads, writes)
        return self.op(eng, lambda e: e.tensor_copy(out=out, in_=in_), reads, writes)

    def reduce(self, eng, out, in_, op, reads=(), writes=(), axis=None):
        ax = axis or AX.X
        return self.op(eng, lambda e: e.tensor_reduce(out=out, in_=in_, axis=ax, op=op), reads, writes)

    def mm(self, out, lhsT, rhs, start, stop, reads=(), writes=()):
        return self.op("pe", lambda e: e.matmul(out, lhsT=lhsT, rhs=rhs, start=start, stop=stop), reads, writes)

    def transpose(self, out, in_, ident, reads=(), writes=()):
        return self.op("pe", lambda e: e.transpose(out, in_, ident), reads, writes)

    def memset(self, eng, ap, val, writes=()):
        return self.op(eng, lambda e: e.memset(ap, val), (), writes)

    def recip(self, out, in_, reads=(), writes=()):
        return self.op("dve", lambda e: e.reciprocal(out=out, in_=in_), reads, writes)

    def max8(self, out, in_, reads=(), writes=()):
        return self.op("dve", lambda e: e.max(out=out, in_=in_), reads, writes)

    def barrier(self):
        toks = []
        for E in self.CE:
            st = self.streams[E]
            for i in range(len(st) - 1, -1, -1):
                if st[i]["dma"] is None and st[i]["fn"] is not None:
                    st[i]["sig"] = True
                    toks.append(("c", E, i))
                    break
        for q, vals in self.slots.items():
            for s, v in enumerate(vals):
                if v > 0:
                    toks.append(("d", (q, s), v))
        for E in self.streams:
            self.streams[E].append(dict(fn=None, waits=list(toks), sig=False, dma=None))

    def finalize(self):
        nc = self.nc
        self.barrier()
        cnt = {}
        for E in self.CE:
            c = 0
            arr = []
            for ent in self.streams[E]:
                if ent["sig"]:
                    c += 1
                arr.append(c)
            cnt[E] = arr
        sems = {}
        for E in self.CE:
            sems[("c", E)] = nc.alloc_semaphore(f"s_{E}")
        for q, vals in self.slots.items():
            for s in range(len(vals)):
                sems[("d", (q, s))] = nc.alloc_semaphore(f"d_{q}{s}")

        def run(E):
            def body(e):
                waited = {}
                for ent in self.streams[E]:
                    for t in ent["waits"]:
                        if t[0] == "c":
                            key = ("c", t[1])
                            val = cnt[t[1]][t[2]]
                        else:
                            key = ("d", t[1])
                            val = t[2]
                        if waited.get(key, 0) >= val:
                            continue
                        waited[key] = val
                        e.wait_ge(sems[key], val)
                    if ent["fn"] is None:
                        continue
                    ins = ent["fn"](e)
                    if ent["dma"] is not None:
                        ins.then_inc(sems[("d", ent["dma"])], 16)
                    elif ent["sig"]:
                        ins.then_inc(sems[("c", E)], 1)
            return body

        with nc.Block() as block:
            block.sync(run("sp"))
            block.scalar(run("act"))
            block.gpsimd(run("pool"))
            block.tensor(run("pe"))
            block.vector(run("dve"))
        return nc

D = 1024
NH = 12
HD = 64
NMH = 4
MEM = 256
DFF = 2816
NJ = DFF // 128
EPS = 1e-6
FOX_IN = 3 * 768 + 12 + 256
MLA_IN = 256 + 128 + 32 + 256
MOBA_IN = 3 * 768 + 256
NEG = -32768.0
TWO_PI = 6.283185307179586
CW1 = 6.28125
CW2 = TWO_PI - CW1


class Ring:
    def __init__(self, P, name, n, shape, dtype):
        self.items = [(P.sbuf(f"{name}{i}", shape, dtype), Buf(f"{name}{i}")) for i in range(n)]
        self.i = 0

    def next(self):
        it = self.items[self.i % len(self.items)]
        self.i += 1
        return it


def bc_mid(ap, n):
    return ap.unsqueeze(1).broadcast_to([ap.shape[0], n, ap.shape[1]])


def bc_last(ap, n):
    return ap.unsqueeze(2).broadcast_to([ap.shape[0], ap.shape[1], n])


def col_groups(n, w=512):
    return [(c, min(c + w, n)) for c in range(0, n, w)]


class Ctx:
    pass


def build_program(S, layer_ids, dbg=False):
    NT = S // 128
    nc = bass.Bass("TRN2", target_bir_lowering=False)
    P = Prog(nc)
    C = Ctx()
    C.S, C.NT, C.nc, C.P = S, NT, nc, P

    def din(name, shape, dt=F32):
        return nc.dram_tensor(name, list(shape), dt, kind="ExternalInput").ap()

    I = {}
    I["x"] = din("x", [S, D])
    I["mem"] = din("mem", [MEM, D])
    I["positions"] = din("positions", [S, 1], I32)
    I["norm_mix"] = din("norm_mix", [4, D])
    I["norm_ffn"] = din("norm_ffn", [4, D])
    I["w_out"] = din("w_out", [4, D, D])
    I["mem_norm"] = din("mem_norm", [D])
    I["w_mem_kv"] = din("w_mem_kv", [D, 512])
    I["mem_q_gain"] = din("mem_q_gain", [4, HD])
    I["mem_k_gain"] = din("mem_k_gain", [4, HD])
    I["fox_w_in"] = din("fox_w_in", [2, D, FOX_IN])
    I["fox_b_f"] = din("fox_b_f", [2, NH])
    I["fox_q_gain"] = din("fox_q_gain", [2, HD])
    I["fox_k_gain"] = din("fox_k_gain", [2, HD])
    I["mla_w_in"] = din("mla_w_in", [1, D, MLA_IN])
    I["mla_qa_norm"] = din("mla_qa_norm", [1, 256])
    I["mla_kva_norm"] = din("mla_kva_norm", [1, 128])
    I["mla_w_q_up"] = din("mla_w_q_up", [1, 256, NH * 96])
    I["mla_w_kv_up"] = din("mla_w_kv_up", [1, 128, NH * 128])
    I["mla_q_gain"] = din("mla_q_gain", [1, 96])
    I["mla_k_gain"] = din("mla_k_gain", [1, 96])
    I["moba_w_in"] = din("moba_w_in", [1, D, MOBA_IN])
    I["moba_q_gain"] = din("moba_q_gain", [1, HD])
    I["moba_k_gain"] = din("moba_k_gain", [1, HD])
    I["ffn_w_up"] = din("ffn_w_up", [4, D, 2 * DFF])
    I["ffn_conv_w"] = din("ffn_conv_w", [4, 3, 2 * DFF])
    I["ffn_conv_b"] = din("ffn_conv_b", [4, 2 * DFF])
    I["ffn_w_down"] = din("ffn_w_down", [4, DFF, D])
    I["c_ident"] = din("c_ident", [128, 128], BF16)
    I["c_identf"] = din("c_identf", [128, 128])
    I["c_tri"] = din("c_tri", [128, 128])
    I["c_maskneg"] = din("c_maskneg", [128, 128], BF16)
    I["c_sel"] = din("c_sel", [19, 16 * 128], BF16)
    I["c_selS"] = din("c_selS", [19, S], BF16)
    I["c_invfreq"] = din("c_invfreq", [16])
    I["c_slopes"] = din("c_slopes", [NH])
    C.I = I
    y = nc.dram_tensor("y", [S, D], F32, kind="ExternalOutput").ap()
    C.y = y
    C.b_y = [Buf(f"y{t}") for t in range(NT)]
    C.QT = nc.dram_tensor("QT", [NH * 96, S], BF16).ap()
    C.KT = nc.dram_tensor("KT", [NH * 96, S], BF16).ap()
    C.QmT = nc.dram_tensor("QmT", [NMH * HD, S], BF16).ap()
    C.V = nc.dram_tensor("Vs", [S, NH * HD], BF16).ap()
    C.AUX = nc.dram_tensor("AUX", [NH, 19, S], BF16).ap()
    C.OT = nc.dram_tensor("OT", [D, S], BF16).ap()
    C.b_QT, C.b_KT, C.b_QmT, C.b_V, C.b_AUX, C.b_OT = (Buf(n) for n in ("QT", "KT", "QmT", "V", "AUX", "OT"))
    C.dbg = {}
    C.PS = []
    for i in range(8):
        t = nc.alloc_psum_tensor(f"psb{i}", [128, 512], F32)
        C.PS.append((t[:], Buf(f"ps{i}")))
    C.bK = Buf("consts")
    C.ident = P.sbuf("ident", [128, 128], BF16)
    C.identf = P.sbuf("identf", [128, 128], F32)
    C.tri = P.sbuf("tri", [128, 128], F32)
    C.maskneg = P.sbuf("maskneg", [128, 128], BF16)
    C.sel = P.sbuf("sel", [19, 16 * 128], BF16)
    C.ones_bf = P.sbuf("ones_bf", [128, 128], BF16)
    C.ones_f = P.sbuf("ones_f", [128, 128], F32)
    C.invf = P.sbuf("invf", [128, 16], F32)
    C.slopes = P.sbuf("slopes", [128, NH], F32)
    P.dma("sp", C.ident, I["c_ident"], writes=[C.bK])
    P.dma("sp", C.identf, I["c_identf"], writes=[C.bK])
    P.dma("sp", C.tri, I["c_tri"], writes=[C.bK])
    P.dma("sp", C.maskneg, I["c_maskneg"], writes=[C.bK])
    P.dma("sp", C.sel, I["c_sel"], writes=[C.bK])
    P.dma("sp", C.invf, I["c_invfreq"].partition_broadcast(128), writes=[C.bK])
    P.dma("sp", C.slopes, I["c_slopes"].partition_broadcast(128), writes=[C.bK])
    P.memset("dve", C.ones_bf, 1.0, writes=[C.bK])
    P.memset("dve", C.ones_f, 1.0, writes=[C.bK])
    C.kmem_raw = P.sbuf("kmem_raw", [128, 2, 256], F32)
    C.b_kmem = Buf("kmem_raw")
    C.vmaug = P.sbuf("vmaug", [128, 2, NMH, 128], BF16)
    C.b_vmaug = Buf("vmaug")
    C.posf = P.sbuf("posf", [128, NT], F32)
    C.b_posf = Buf("posf")
    C.cos = P.sbuf("cos", [128, NT, 16], F32)
    C.sin = P.sbuf("sin", [128, NT, 16], F32)
    C.b_cs = Buf("cossin")
    C.bias_tab = P.sbuf("bias_tab", [128, NT, NH], F32)
    C.b_bias = Buf("bias_tab")
    C.kmT = P.sbuf("kmT", [64, NMH, MEM], BF16)
    C.b_kmT = Buf("kmT")
    P.barrier()
    phase0(C)
    import os as _os
    _stop = _os.environ.get("KSTOP", "Z")
    for li in layer_ids:
        if _stop == "0":
            break
        kind, j = li % 3, li // 3
        xsrc = I["x"] if li == layer_ids[0] else y
        P.barrier()
        m = P.mark()
        phaseA(C, li, kind, j, xsrc)
        P.release(m)
        if _stop == "A":
            break
        P.barrier()
        m = P.mark()
        phaseB(C, kind)
        P.release(m)
        if _stop == "B":
            break
        P.barrier()
        m = P.mark()
        phaseC(C, li, xsrc)
        P.release(m)
        if _stop == "C":
            break
        P.barrier()
        m = P.mark()
        phaseD(C, li)
        P.release(m)
    P.finalize()
    return nc

def rms_rows(C, xt, bx, W, gb, bg, out, bout, junk, bjunk, st, bst):
    P = C.P
    P.act(junk, xt, AF.Square, reads=[bx], writes=[bjunk])
    P.reduce("dve", st[:, 0:1], junk, ALU.add, reads=[bjunk], writes=[bst])
    P.ts("dve", st[:, 1:2], st[:, 0:1], 1.0 / W, EPS, ALU.mult, ALU.add, reads=[bst], writes=[bst])
    P.act(st[:, 2:3], st[:, 1:2], AF.Ln, reads=[bst], writes=[bst])
    P.act(st[:, 3:4], st[:, 2:3], AF.Exp, scale=-0.5, reads=[bst], writes=[bst])
    P.stt(out, xt, st[:, 3:4], gb, ALU.mult, ALU.mult, reads=[bx, bst, bg], writes=[bout])


def head_norm(C, eng, src3, bsrc, nh, hd, gain3, bgain, out3, bout, junk3, bjunk, st, bst):
    P = C.P
    P.act(junk3, src3, AF.Square, reads=[bsrc], writes=[bjunk])
    P.reduce("dve", st[:, 0, 0:nh], junk3, ALU.add, reads=[bjunk], writes=[bst])
    P.ts("dve", st[:, 1, 0:nh], st[:, 0, 0:nh], 1.0 / hd, EPS, ALU.mult, ALU.add, reads=[bst], writes=[bst])
    P.act(st[:, 2, 0:nh], st[:, 1, 0:nh], AF.Ln, reads=[bst], writes=[bst])
    P.act(st[:, 3, 0:nh], st[:, 2, 0:nh], AF.Exp, scale=-0.5, reads=[bst], writes=[bst])
    P.tt(eng, junk3, src3, bc_last(st[:, 3, 0:nh], hd), ALU.mult, reads=[bsrc, bst], writes=[bjunk])
    P.tt(eng, out3, junk3, gain3, ALU.mult, reads=[bjunk, bgain], writes=[bout])


def transpose_to(C, src_bf, bsrc, nblk, bank, dst, bdst, rows=128, blkw=128, evac="act"):
    P = C.P
    ps, bps = C.PS[bank]
    psb = ps.bitcast(BF16)
    done = 0
    while done < nblk:
        n = min(8, nblk - done)
        for b in range(n):
            P.transpose(psb[0:blkw, b * 128:(b + 1) * 128], src_bf[:, (done + b) * blkw:(done + b + 1) * blkw],
                        C.ident, reads=[bsrc, C.bK], writes=[bps])
        P.copy(evac, dst[0:blkw, done:done + n, :], psb[0:blkw, 0:n * 128].rearrange("p (a b) -> p a b", b=128),
               reads=[bps], writes=[bdst])
        done += n


def phase0(C):
    P, I, NT = C.P, C.I, C.NT
    import os as _os
    _p0 = _os.environ.get("KP0", "z")
    if _p0 == "a":
        return
    m = P.mark()
    posi = P.sbuf("posi", [128, NT, 1], I32)
    b_posi = Buf("posi")
    P.dma("sp", posi, I["positions"].rearrange("(t p) o -> p t o", p=128), writes=[b_posi], allow_slow_non_contiguous=True)
    P.copy("dve", C.posf, posi[:, :, 0], reads=[b_posi], writes=[C.b_posf])
    if _p0 == "b":
        return
    ang = P.sbuf("ang", [128, NT, 16], F32)
    b_ang = Buf("ang")
    tmp = P.sbuf("rtmp", [128, NT, 16], F32)
    b_tmp = Buf("rtmp")
    tmp2 = P.sbuf("rtmp2", [128, NT, 16], F32)
    b_tmp2 = Buf("rtmp2")
    ki = P.sbuf("rki", [128, NT, 16], I32)
    b_ki = Buf("rki")
    P.tt("dve", ang, bc_last(C.posf, 16), bc_mid(C.invf, NT), ALU.mult, reads=[C.b_posf, C.bK], writes=[b_ang])
    for which, dst in ((0, C.sin), (1, C.cos)):
        src = ang
        bsrc = b_ang
        if which == 1:
            P.ts("dve", tmp2, ang, math.pi / 2, None, ALU.add, reads=[b_ang], writes=[b_tmp2])
            src, bsrc = tmp2, b_tmp2
        P.ts("dve", tmp, src, 1.0 / TWO_PI, None, ALU.mult, reads=[bsrc], writes=[b_tmp])
        P.copy("dve", ki, tmp, reads=[b_tmp], writes=[b_ki])
        P.copy("dve", tmp, ki, reads=[b_ki], writes=[b_tmp])
        P.stt(dst, tmp, -CW1, src, ALU.mult, ALU.add, reads=[b_tmp, bsrc], writes=[C.b_cs])
        P.stt(dst, tmp, -CW2, dst, ALU.mult, ALU.add, reads=[b_tmp, C.b_cs], writes=[C.b_cs])
        P.ts("dve", dst, dst, math.pi, -math.pi, ALU.min, ALU.max, reads=[C.b_cs], writes=[C.b_cs])
        P.act(dst, dst, AF.Sin, reads=[C.b_cs], writes=[C.b_cs])
    if _p0 == "c":
        return
    wkv = P.sbuf("wkv", [128, 8, 512], BF16)
    b_wkv = Buf("wkv")
    for k in range(8):
        P.dma("pool", wkv[:, k, :], I["w_mem_kv"][k * 128:(k + 1) * 128, :], writes=[b_wkv])
    gb = P.sbuf("g_mem", [128, D], F32)
    b_gb = Buf("g_mem")
    P.dma("sp", gb, I["mem_norm"].partition_broadcast(128), writes=[b_gb])
    P.memset("dve", C.vmaug, 1.0, writes=[C.b_vmaug])
    xr = Ring(P, "m_x", 2, [128, D], F32)
    jr = Ring(P, "m_junk", 2, [128, D], F32)
    sr = Ring(P, "m_st", 2, [128, 4], F32)
    hr = Ring(P, "m_h", 2, [128, D], BF16)
    tr = Ring(P, "m_hT", 2, [128, 8, 128], BF16)
    for mt in range(2):
        if _p0 == "d1":
            break
        xt, bx = xr.next()
        junk, bj = jr.next()
        st, bst = sr.next()
        hb, bh = hr.next()
        hT, bhT = tr.next()
        P.dma("sp", xt, I["mem"][mt * 128:(mt + 1) * 128, :], writes=[bx])
        rms_rows(C, xt, bx, D, gb, b_gb, hb, bh, junk, bj, st, bst)
        if _p0 == "d2":
            break
        transpose_to(C, hb, bh, 8, mt, hT, bhT)
        if _p0 == "d3":
            break
        ps, bps = C.PS[2 + mt]
        for k in range(8):
            P.mm(ps, hT[:, k, :], wkv[:, k, :], k == 0, k == 7, reads=[bhT, b_wkv], writes=[bps])
        if _p0 == "d4":
            break
        P.copy("act", C.kmem_raw[:, mt, :], ps[:, 0:256], reads=[bps], writes=[C.b_kmem])
        if _p0 == "d5":
            continue
        P.copy("act", C.vmaug[:, mt, :, 0:64], ps[:, 256:512].rearrange("p (h d) -> p h d", d=64),
               reads=[bps], writes=[C.b_vmaug])
    P.release(m)

def load_gain(C, name, src_row, W, scale=None):
    P = C.P
    t = P.sbuf(name, [128, W], F32)
    b = Buf(name)
    P.dma("sp", t, src_row.partition_broadcast(128), writes=[b])
    if scale is not None:
        P.ts("dve", t, t, float(scale), None, ALU.mult, reads=[b], writes=[b])
    return t, b


def phaseA(C, li, kind, j, xsrc):
    P, I, NT, S = C.P, C.I, C.NT, C.S
    if kind == 0:
        w_in, NIN = I["fox_w_in"][j], FOX_IN
        q_gain, k_gain = I["fox_q_gain"][j], I["fox_k_gain"][j]
        QM0 = 2316
    elif kind == 1:
        w_in, NIN = I["mla_w_in"][j], MLA_IN
        QM0 = 416
    else:
        w_in, NIN = I["moba_w_in"][j], MOBA_IN
        q_gain, k_gain = I["moba_q_gain"][j], I["moba_k_gain"][j]
        QM0 = 2304
    DK = 96 if kind == 1 else 64
    wbf = P.sbuf("w_in", [128, 8, NIN], BF16)
    b_w = Buf("w_in")
    for k in range(8):
        P.dma("pool", wbf[:, k, :], w_in[k * 128:(k + 1) * 128, :], writes=[b_w])
    gmix, b_gmix = load_gain(C, "g_mix", I["norm_mix"][li], D)
    gmq, b_gmq = load_gain(C, "g_mq", I["mem_q_gain"][li], HD, HD ** -0.5)
    gmk, b_gmk = load_gain(C, "g_mk", I["mem_k_gain"][li], HD)
    if kind != 1:
        gq, b_gq = load_gain(C, "g_q", q_gain, HD, HD ** -0.5)
        gk, b_gk = load_gain(C, "g_k", k_gain, HD)
    else:
        gq, b_gq = load_gain(C, "g_q", I["mla_q_gain"][j], 96, 96 ** -0.5)
        gk, b_gk = load_gain(C, "g_k", I["mla_k_gain"][j], 96)
        gqa, b_gqa = load_gain(C, "g_qa", I["mla_qa_norm"][j], 256)
        gkva, b_gkva = load_gain(C, "g_kva", I["mla_kva_norm"][j], 128)
        wq = P.sbuf("w_qup", [128, 2, NH * 96], BF16)
        b_wq = Buf("w_qup")
        P.dma("pool", wq, I["mla_w_q_up"][j].rearrange("(c p) n -> p c n", p=128), writes=[b_wq])
        wkv = P.sbuf("w_kvup", [128, NH * 128], BF16)
        b_wkv = Buf("w_kvup")
        P.dma("pool", wkv, I["mla_w_kv_up"][j], writes=[b_wkv])
    if kind == 0:
        bfb, b_bfb = load_gain(C, "b_f", I["fox_b_f"][j], NH)
        carry = P.sbuf("carry", [1, NH], F32)
        b_carry = Buf("carry")
        P.memset("dve", carry, 0.0, writes=[b_carry])
    if kind == 2:
        P.tt("dve", C.bias_tab, bc_last(C.posf, NH), bc_mid(C.slopes, NT), ALU.mult,
             reads=[C.b_posf, C.bK], writes=[C.b_bias])
        ksum = P.sbuf("ksum", [128, 6, NT], F32)
        b_ksum = Buf("ksum")
        kmean = P.sbuf("kmean", [128, 6, 32], BF16)
        b_kmean = Buf("kmean")
        P.memset("dve", kmean, 0.0, writes=[b_kmean])
        nmrows = [P.sbuf("nmrows0", [128, S], BF16), P.sbuf("nmrows1", [64, S], BF16)]
        b_nmrows = Buf("nmrows")
    if kind != 1:
        prow = P.sbuf("prow", [36, S], BF16)
        b_prow = Buf("prow")
    mj = P.sbuf("mk_junk", [128, NMH, HD], F32)
    b_mj = Buf("mk_junk")
    mst = P.sbuf("mk_st", [128, 4, 16], F32)
    b_mst = Buf("mk_st")
    mkn = P.sbuf("mk_n", [128, NMH * HD], BF16)
    b_mkn = Buf("mk_n")
    for mt in range(2):
        head_norm(C, "dve", C.kmem_raw[:, mt, :].rearrange("p (h d) -> p h d", d=HD), C.b_kmem, NMH, HD,
                  bc_mid(gmk, NMH), b_gmk, mkn.rearrange("p (h d) -> p h d", d=HD), b_mkn, mj, b_mj, mst, b_mst)
        ps, bps = C.PS[7]
        psb = ps.bitcast(BF16)
        for h in range(NMH):
            P.transpose(psb[0:64, h * 128:(h + 1) * 128], mkn[:, h * 64:(h + 1) * 64], C.ident,
                        reads=[b_mkn, C.bK], writes=[bps])
        P.copy("act", C.kmT[:, :, mt * 128:(mt + 1) * 128], psb[0:64, 0:512].rearrange("p (h m) -> p h m", m=128),
               reads=[bps], writes=[C.b_kmT])
    xr = Ring(P, "a_x", 2, [128, D], F32)
    jr = Ring(P, "a_junk", 2, [128, D], F32)
    sr = Ring(P, "a_st", 2, [128, 4], F32)
    hr = Ring(P, "a_h", 2, [128, D], BF16)
    tr = Ring(P, "a_hT", 2, [128, 8, 128], BF16)
    pr = Ring(P, "a_p", 2, [128, NIN], F32)
    NQ = NH * DK
    qnr = Ring(P, "a_qn", 2, [128, NQ], BF16)
    knr = Ring(P, "a_kn", 2, [128, NQ], BF16)
    qmr = Ring(P, "a_qm", 2, [128, 256], BF16)
    vbr = Ring(P, "a_vb", 2, [128, 768], BF16)
    hjq = Ring(P, "a_hjq", 1, [128, NQ], F32)
    hjk = Ring(P, "a_hjk", 1, [128, NQ], F32)
    hjm = Ring(P, "a_hjm", 1, [128, 256], F32)
    hsq = Ring(P, "a_hsq", 2, [128, 4, 16], F32)
    hsk = Ring(P, "a_hsk", 2, [128, 4, 16], F32)
    hsm = Ring(P, "a_hsm", 2, [128, 4, 16], F32)
    NBQ = NQ // 128 if kind != 1 else NH
    BW = 128 if kind != 1 else 96
    qts = Ring(P, "a_qts", 2, [128, NBQ, 512], BF16)
    kts = Ring(P, "a_kts", 2, [128, NBQ, 512], BF16)
    qmts = Ring(P, "a_qmts", 2, [128, 2, 512], BF16)
    small = Ring(P, "a_small", 2, [128, 8, 16], F32)
    pcs = Ring(P, "a_pcs", 2, [128, NH * 3], BF16)
    if kind == 1:
        qlr = Ring(P, "a_ql", 2, [128, 256], BF16)
        kvlr = Ring(P, "a_kvl", 2, [128, 128], BF16)
        qlT = Ring(P, "a_qlT", 2, [128, 2, 128], BF16)
        kvT = Ring(P, "a_kvT", 2, [128, 1, 128], BF16)
        qfr = Ring(P, "a_qf", 1, [128, NH, 96], F32)
        kvfr = Ring(P, "a_kvf", 1, [128, NH, 128], F32)
        kfr = Ring(P, "a_kf", 1, [128, NH, 96], F32)
        rtmp = Ring(P, "a_rt", 2, [128, 4, NH, 16], F32)
        krr = Ring(P, "a_kr", 2, [128, 32], F32)
    if kind == 2:
        gater = Ring(P, "a_gate", 2, [128, NH, 16], F32)
        top8 = Ring(P, "a_top8", 2, [128, NH, 8], F32)
        nmr = Ring(P, "a_nm", 2, [128, NH * 16], BF16)
        qtc = Ring(P, "a_qtc", 2, [128, 6, 128], BF16)
    groups = col_groups(NIN)
    gbank = [2, 3, 4]
    gi = 0
    cur_qts = cur_kts = cur_qmts = None
    gstate = dict(gi=0)

    def front(tt):
        tok = slice(tt * 128, (tt + 1) * 128)
        xt, bx = xr.next()
        junk, bj = jr.next()
        st, bst = sr.next()
        hb, bh = hr.next()
        hT, bhT = tr.next()
        psb_, bp = pr.next()
        P.dma("sp", xt, xsrc[tok, :], reads=[C.b_y[tt]], writes=[bx])
        rms_rows(C, xt, bx, D, gmix, b_gmix, hb, bh, junk, bj, st, bst)
        transpose_to(C, hb, bh, 8, tt % 2, hT, bhT)
        for (c0, c1) in groups:
            ps, bps = C.PS[gbank[gstate["gi"] % 3]]
            gstate["gi"] += 1
            for k in range(8):
                P.mm(ps[:, 0:c1 - c0], hT[:, k, :], wbf[:, k, c0:c1], k == 0, k == 7, reads=[bhT, b_w], writes=[bps])
            P.copy("act" if gstate["gi"] % 2 else "dve", psb_[:, c0:c1], ps[:, 0:c1 - c0], reads=[bps], writes=[bp])
        return dict(psb_=psb_, bp=bp, junk=junk, bj=bj, st=st, bst=bst)

    fstate = {0: front(0)}
    for tt in range(NT):
        if tt + 1 < NT:
            fstate[tt + 1] = front(tt + 1)
        F_ = fstate.pop(tt)
        psb_, bp, junk, bj, st, bst = F_["psb_"], F_["bp"], F_["junk"], F_["bj"], F_["st"], F_["bst"]
        gi = gstate["gi"]
        tok = slice(tt * 128, (tt + 1) * 128)
        sub = tt % 4
        if sub == 0:
            cur_qts, cur_kts, cur_qmts = qts.next(), kts.next(), qmts.next()
        (qT, bqT), (kT, bkT), (qmT, bqmT) = cur_qts, cur_kts, cur_qmts
        qmn, bqmn = qmr.next()
        j3, bj3 = hjm.next()
        s3, bs3 = hsm.next()
        head_norm(C, "pool", psb_[:, QM0:QM0 + 256].rearrange("p (h d) -> p h d", d=HD), bp, NMH, HD,
                  bc_mid(gmq, NMH), b_gmq, qmn.rearrange("p (h d) -> p h d", d=HD), bqmn,
                  j3.rearrange("p (h d) -> p h d", d=HD), bj3, s3, bs3)
        if kind != 1:
            qn, bqn = qnr.next()
            kn, bkn = knr.next()
            vb, bvb = vbr.next()
            jq, bjq = hjq.next()
            jk, bjk = hjk.next()
            sq, bsq = hsq.next()
            sk, bsk = hsk.next()
            head_norm(C, "dve", psb_[:, 0:768].rearrange("p (h d) -> p h d", d=HD), bp, NH, HD,
                      bc_mid(gq, NH), b_gq, qn.rearrange("p (h d) -> p h d", d=HD), bqn,
                      jq.rearrange("p (h d) -> p h d", d=HD), bjq, sq, bsq)
            head_norm(C, "pool", psb_[:, 768:1536].rearrange("p (h d) -> p h d", d=HD), bp, NH, HD,
                      bc_mid(gk, NH), b_gk, kn.rearrange("p (h d) -> p h d", d=HD), bkn,
                      jk.rearrange("p (h d) -> p h d", d=HD), bjk, sk, bsk)
            P.copy("act", vb, psb_[:, 1536:2304], reads=[bp], writes=[bvb])
            P.dma("sp", C.V[tok, :], vb, reads=[bvb], writes=[C.b_V])
        else:
            ql, bql = qlr.next()
            kvl, bkvl = kvlr.next()
            rms_rows(C, psb_[:, 0:256], bp, 256, gqa, b_gqa, ql, bql, junk[:, 0:256], bj, st, bst)
            rms_rows(C, psb_[:, 256:384], bp, 128, gkva, b_gkva, kvl, bkvl, junk[:, 256:384], bj, st, bst)
            qlt, bqlt = qlT.next()
            kvt, bkvt = kvT.next()
            transpose_to(C, ql, bql, 2, 7, qlt, bqlt)
            transpose_to(C, kvl, bkvl, 1, 7, kvt, bkvt)
            qf, bqf = qfr.next()
            kvf, bkvf = kvfr.next()
            kf, bkf = kfr.next()
            qf2 = qf.rearrange("p h d -> p (h d)")
            kvf2 = kvf.rearrange("p h d -> p (h d)")
            for (c0, c1) in col_groups(NH * 96):
                ps, bps = C.PS[gbank[gi % 3]]
                gi += 1
                for k in range(2):
                    P.mm(ps[:, 0:c1 - c0], qlt[:, k, :], wq[:, k, c0:c1], k == 0, k == 1, reads=[bqlt, b_wq], writes=[bps])
                P.copy("act" if gi % 2 else "dve", qf2[:, c0:c1], ps[:, 0:c1 - c0], reads=[bps], writes=[bqf])
            for (c0, c1) in col_groups(NH * 128):
                ps, bps = C.PS[gbank[gi % 3]]
                gi += 1
                P.mm(ps[:, 0:c1 - c0], kvt[:, 0, :], wkv[:, c0:c1], True, True, reads=[bkvt, b_wkv], writes=[bps])
                P.copy("act" if gi % 2 else "dve", kvf2[:, c0:c1], ps[:, 0:c1 - c0], reads=[bps], writes=[bkvf])
            gstate["gi"] = gi
            cosb = C.cos[:, tt, :]
            sinb = C.sin[:, tt, :]
            rt, brt = rtmp.next()
            x1 = qf[:, :, 64:80]
            x2 = qf[:, :, 80:96]
            P.tt("dve", rt[:, 0], x1, bc_mid(cosb, NH), ALU.mult, reads=[bqf, C.b_cs], writes=[brt])
            P.tt("dve", rt[:, 1], x2, bc_mid(sinb, NH), ALU.mult, reads=[bqf, C.b_cs], writes=[brt])
            P.tt("dve", rt[:, 2], x1, bc_mid(sinb, NH), ALU.mult, reads=[bqf, C.b_cs], writes=[brt])
            P.tt("dve", rt[:, 3], x2, bc_mid(cosb, NH), ALU.mult, reads=[bqf, C.b_cs], writes=[brt])
            P.tt("dve", x1, rt[:, 0], rt[:, 1], ALU.subtract, reads=[brt], writes=[bqf])
            P.tt("dve", x2, rt[:, 2], rt[:, 3], ALU.add, reads=[brt], writes=[bqf])
            kr, bkr = krr.next()
            k1 = psb_[:, 384:400]
            k2 = psb_[:, 400:416]
            P.tt("pool", rt[:, 0, 0], k1, cosb, ALU.mult, reads=[bp, C.b_cs, brt], writes=[brt])
            P.tt("pool", rt[:, 1, 0], k2, sinb, ALU.mult, reads=[bp, C.b_cs], writes=[brt])
            P.tt("pool", rt[:, 2, 0], k1, sinb, ALU.mult, reads=[bp, C.b_cs], writes=[brt])
            P.tt("pool", rt[:, 3, 0], k2, cosb, ALU.mult, reads=[bp, C.b_cs], writes=[brt])
            P.tt("pool", kr[:, 0:16], rt[:, 0, 0], rt[:, 1, 0], ALU.subtract, reads=[brt], writes=[bkr])
            P.tt("pool", kr[:, 16:32], rt[:, 2, 0], rt[:, 3, 0], ALU.add, reads=[brt], writes=[bkr])
            P.copy("pool", kf[:, :, 0:64], kvf[:, :, 0:64], reads=[bkvf], writes=[bkf])
            P.copy("pool", kf[:, :, 64:96], bc_mid(kr, NH), reads=[bkr], writes=[bkf])
            qn, bqn = qnr.next()
            kn, bkn = knr.next()
            vb, bvb = vbr.next()
            jq, bjq = hjq.next()
            jk, bjk = hjk.next()
            sq, bsq = hsq.next()
            sk, bsk = hsk.next()
            head_norm(C, "dve", qf, bqf, NH, 96, bc_mid(gq, NH), b_gq, qn.rearrange("p (h d) -> p h d", d=96), bqn,
                      jq.rearrange("p (h d) -> p h d", d=96), bjq, sq, bsq)
            head_norm(C, "pool", kf, bkf, NH, 96, bc_mid(gk, NH), b_gk, kn.rearrange("p (h d) -> p h d", d=96), bkn,
                      jk.rearrange("p (h d) -> p h d", d=96), bjk, sk, bsk)
            P.copy("act", vb.rearrange("p (h d) -> p h d", d=64), kvf[:, :, 64:128], reads=[bkvf], writes=[bvb])
            P.dma("sp", C.V[tok, :], vb, reads=[bvb], writes=[C.b_V])
        tsl = slice(sub * 128, (sub + 1) * 128)
        transpose_to(C, qn, bqn, NBQ, 5, qT[:, :, tsl], bqT, blkw=BW, evac="act")
        transpose_to(C, kn, bkn, NBQ, 6, kT[:, :, tsl], bkT, blkw=BW, evac="dve")
        transpose_to(C, qmn, bqmn, 2, 7, qmT[:, :, tsl], bqmT, evac="act")
        sm, bsm = small.next()
        if kind == 0:
            P.tt("dve", sm[:, 0, 0:NH], psb_[:, 2304:2316], bfb, ALU.add, reads=[bp, b_bfb], writes=[bsm])
            P.act(sm[:, 1, 0:NH], sm[:, 0, 0:NH], AF.Exp, scale=-1.0, reads=[bsm], writes=[bsm])
            P.ts("dve", sm[:, 2, 0:NH], sm[:, 1, 0:NH], 1.0, None, ALU.add, reads=[bsm], writes=[bsm])
            P.act(sm[:, 3, 0:NH], sm[:, 2, 0:NH], AF.Ln, reads=[bsm], writes=[bsm])
            ps, bps = C.PS[7]
            P.mm(ps[:, 0:NH], C.tri, sm[:, 3, 0:NH], True, False, reads=[C.bK, bsm], writes=[bps])
            P.mm(ps[:, 0:NH], C.ones_f[0:1, :], carry, False, True, reads=[C.bK, b_carry], writes=[bps])
            P.mm(ps[0:1, 16:16 + NH], C.ones_f[:, 0:1], sm[:, 3, 0:NH], True, False, reads=[C.bK, bsm], writes=[bps])
            P.mm(ps[0:1, 16:16 + NH], C.ones_f[0:1, 0:1], carry, False, True, reads=[C.bK, b_carry], writes=[bps])
            P.copy("dve", C.bias_tab[:, tt, :], ps[:, 0:NH], reads=[bps], writes=[C.b_bias])
            P.copy("dve", carry, ps[0:1, 16:16 + NH], reads=[bps], writes=[b_carry])
        if kind != 1:
            pc, bpc = pcs.next()
            pc3 = pc.rearrange("p (h r) -> p h r", r=3)
            P.ts("dve", sm[:, 4, 0:NH], C.bias_tab[:, tt, :], -1.0, None, ALU.mult, reads=[C.b_bias], writes=[bsm])
            P.copy("dve", pc3[:, :, 0], sm[:, 4, 0:NH], reads=[bsm], writes=[bpc])
            P.tt("dve", sm[:, 5, 0:NH], sm[:, 4, 0:NH], pc3[:, :, 0], ALU.subtract, reads=[bsm, bpc], writes=[bsm])
            P.copy("dve", pc3[:, :, 1], sm[:, 5, 0:NH], reads=[bsm], writes=[bpc])
            P.tt("dve", sm[:, 6, 0:NH], sm[:, 5, 0:NH], pc3[:, :, 1], ALU.subtract, reads=[bsm, bpc], writes=[bsm])
            P.copy("dve", pc3[:, :, 2], sm[:, 6, 0:NH], reads=[bsm], writes=[bpc])
            ps, bps = C.PS[7]
            psb = ps.bitcast(BF16)
            P.transpose(psb[0:36, 256:384], pc, C.ident, reads=[bpc, C.bK], writes=[bps])
            P.copy("act", prow[:, tok], psb[0:36, 256:384], reads=[bps], writes=[b_prow])
        if kind == 2:
            ps, bps = C.PS[7]
            for pj in range(6):
                P.mm(ps[:, 256 + pj * 16:256 + (pj + 1) * 16], kn[:, pj * 128:(pj + 1) * 128], C.ones_bf[:, 0:16], True, True,
                     reads=[bkn, C.bK], writes=[bps])
            P.copy("dve", ksum[:, :, tt], ps[:, 256:352].rearrange("p (a b) -> p a b", b=16)[:, :, 0], reads=[bps], writes=[b_ksum])
            nvalid = tt // 2
            gate, bgate = gater.next()
            t8, bt8 = top8.next()
            nm, bnm = nmr.next()
            nm3 = nm.rearrange("p (h n) -> p h n", n=16)
            P.memset("dve", gate, -1e30, writes=[bgate])
            if nvalid > 0:
                qc, bqc = qtc.next()
                P.copy("pool", qc, qT[:, :, tsl], reads=[bqT], writes=[bqc])
                for pj in range(6):
                    P.mm(ps[:, 64 + pj * 32:64 + (pj + 1) * 32], qc[:, pj, :], kmean[:, pj, :], True, True,
                         reads=[bqc, b_kmean], writes=[bps])
                P.copy("dve", gate[:, :, 0:nvalid],
                       ps[:, 64:64 + NH * 16].rearrange("p (h n) -> p h n", n=16)[:, :, 0:nvalid],
                       reads=[bps], writes=[bgate])
            for h in range(NH):
                P.max8(t8[:, h, :], gate[:, h, :], reads=[bgate], writes=[bt8])
            P.tt("dve", gate, gate, bc_last(t8[:, :, 2], 16), ALU.subtract, reads=[bgate, bt8], writes=[bgate])
            P.ts("dve", gate, gate, 0.0, None, ALU.is_ge, reads=[bgate], writes=[bgate])
            P.ts("dve", nm3, gate, -NEG, NEG, ALU.mult, ALU.add, reads=[bgate], writes=[bnm])
            P.memset("dve", nm3[:, :, nvalid:nvalid + 1], 0.0, writes=[bnm])
            if nvalid + 1 < 16:
                P.memset("dve", nm3[:, :, nvalid + 1:16], NEG, writes=[bnm])
            psb = ps.bitcast(BF16)
            P.transpose(psb[:, 512:640], nm[:, 0:128], C.ident, reads=[bnm, C.bK], writes=[bps])
            P.transpose(psb[0:64, 640:768], nm[:, 128:192], C.ident, reads=[bnm, C.bK], writes=[bps])
            P.copy("act", nmrows[0][:, tok], psb[:, 512:640], reads=[bps], writes=[b_nmrows])
            P.copy("act", nmrows[1][:, tok], psb[0:64, 640:768], reads=[bps], writes=[b_nmrows])
            if tt % 2 == 1:
                n = tt // 2
                P.tt("dve", ksum[:, :, tt], ksum[:, :, tt], ksum[:, :, tt - 1], ALU.add, reads=[b_ksum], writes=[b_ksum])
                P.ts("dve", kmean[0:64, :, n], ksum[0:64, :, tt], 1.0 / 256, None, ALU.mult, reads=[b_ksum], writes=[b_kmean])
                P.ts("dve", kmean[64:128, :, 16 + n], ksum[64:128, :, tt], 1.0 / 256, None, ALU.mult, reads=[b_ksum], writes=[b_kmean])
        if sub == 3:
            c4 = slice((tt - 3) * 128, (tt + 1) * 128)
            if kind != 1:
                P.dma("sp", C.QT[0:768, c4].rearrange("(b p) s -> p b s", p=128), qT, reads=[bqT], writes=[C.b_QT])
                P.dma("sp", C.KT[0:768, c4].rearrange("(b p) s -> p b s", p=128), kT, reads=[bkT], writes=[C.b_KT])
            else:
                P.dma("sp", C.QT[:, c4].rearrange("(b p) s -> p b s", p=96), qT[0:96], reads=[bqT], writes=[C.b_QT])
                P.dma("sp", C.KT[:, c4].rearrange("(b p) s -> p b s", p=96), kT[0:96], reads=[bkT], writes=[C.b_KT])
            P.dma("sp", C.QmT[:, c4].rearrange("(b p) s -> p b s", p=128), qmT, reads=[bqmT], writes=[C.b_QmT])
    if kind != 1:
        for h in range(NH):
            P.dma("sp", C.AUX[h, 0:3, :], prow[3 * h:3 * h + 3, :], reads=[b_prow], writes=[C.b_AUX])
    if kind == 2:
        for h in range(NH):
            src = nmrows[0][16 * h:16 * h + 16, :] if h < 8 else nmrows[1][16 * (h - 8):16 * (h - 8) + 16, :]
            P.dma("sp", C.AUX[h, 3:19, :], src, reads=[b_nmrows], writes=[C.b_AUX])

def phaseB(C, kind):
    P, NT, S = C.P, C.NT, C.S
    NCH = S // 512
    DK = 96 if kind == 1 else 64
    R = {0: 3, 1: 0, 2: 19}[kind]
    qr = Ring(P, "b_q", 2, [96, S], BF16)
    kr = Ring(P, "b_k", 2, [96, S], BF16)
    vr = Ring(P, "b_v", 2, [128, NT, 128], BF16)
    ptr = Ring(P, "b_pt", 3, [128, 512], BF16)
    rdr = Ring(P, "b_rd", 2, [64, 512], F32)
    onr = Ring(P, "b_on", 2, [64, 512], BF16)
    for (v, bv) in vr.items:
        P.memset("pool", v[:, :, 64:128], 1.0, writes=[bv])
    st_banks = [0, 1, 2]
    acc_banks = [3, 4]
    state = dict(si=0, ci=0)

    def load_head(h):
        q, bq = qr.next()
        if h < NH:
            k, bk = kr.next()
            v, bv = vr.next()
            P.dma("sp", q[0:DK, :], C.QT[h * DK:(h + 1) * DK, :], reads=[C.b_QT], writes=[bq])
            P.dma("sp", k[0:DK, :], C.KT[h * DK:(h + 1) * DK, :], reads=[C.b_KT], writes=[bk])
            P.dma("sp", v[:, :, 0:64], C.V[:, h * 64:(h + 1) * 64].rearrange("(t p) d -> p t d", p=128),
                  reads=[C.b_V], writes=[bv])
            if R:
                P.dma("sp", q[64:64 + R, :], C.AUX[h, 0:R, :], reads=[C.b_AUX], writes=[bq])
                P.dma("sp", k[64:64 + R, :], C.I["c_selS"][0:R, :], writes=[bk])
            return dict(q=q, bq=bq, k=k, bk=bk, v=v, bv=bv)
        hm = h - NH
        P.dma("sp", q[0:64, :], C.QmT[hm * 64:(hm + 1) * 64, :], reads=[C.b_QmT], writes=[bq])
        return dict(q=q, bq=bq)

    def run_head(h, L):
        mem = h >= NH
        dk = 64 if mem else DK + R
        steps = []
        for c in range(NCH):
            nk = 2 if mem else 4 * c + 4
            for kt in range(nk):
                steps.append((c, kt, kt == 0, kt == nk - 1))
        LOOK = 2
        pend = []
        accs = {}
        for i in range(len(steps) + LOOK):
            if i < len(steps):
                c, kt, first, last = steps[i]
                jd = -1 if mem else kt - 4 * c
                c0 = 128 * jd if jd > 0 else 0
                qs = L["q"][0:dk, c * 512 + c0:(c + 1) * 512]
                ps, bps = C.PS[st_banks[state["si"] % 3]]
                state["si"] += 1
                pt, bpt = ptr.next()
                if mem:
                    ksl = C.kmT[:, h - NH, kt * 128:(kt + 1) * 128]
                    P.mm(ps[:, c0:512], ksl, qs, True, True, reads=[C.b_kmT, L["bq"]], writes=[bps])
                    P.act(pt[:, c0:512], ps[:, c0:512], AF.Exp, reads=[bps], writes=[bpt])
                else:
                    ksl = L["k"][0:dk, kt * 128:(kt + 1) * 128]
                    more = jd >= 0
                    P.mm(ps[:, c0:512], ksl, qs, True, not more, reads=[L["bk"], L["bq"]], writes=[bps])
                    if jd >= 0:
                        P.mm(ps[:, c0:c0 + 128], C.ident, C.maskneg, False, True, reads=[C.bK], writes=[bps])
                    if R:
                        P.act(pt[:, c0:512], ps[:, c0:512], AF.Exp, bias=C.bias_tab[:, kt, h:h + 1],
                              reads=[bps, C.b_bias], writes=[bpt])
                    else:
                        P.act(pt[:, c0:512], ps[:, c0:512], AF.Exp, reads=[bps], writes=[bpt])
                pend.append((c, kt, first, last, c0, pt, bpt))
            if i >= LOOK:
                c, kt, first, last, c0, pt, bpt = pend.pop(0)
                if first:
                    accs[c] = C.PS[acc_banks[state["ci"] % 2]]
                    state["ci"] += 1
                acc, bacc = accs[c]
                if mem:
                    vsl, bvs = C.vmaug[:, kt, h - NH, :], C.b_vmaug
                else:
                    vsl, bvs = L["v"][:, kt, :], L["bv"]
                P.mm(acc[:, c0:512], vsl, pt[:, c0:512], first, last, reads=[bvs, bpt], writes=[bacc])
                if last:
                    rd, brd = rdr.next()
                    on, bon = onr.next()
                    P.recip(rd, acc[64:128, :], reads=[bacc], writes=[brd])
                    P.tt("dve", on, acc[0:64, :], rd, ALU.mult, reads=[bacc, brd], writes=[bon])
                    P.dma("pool", C.OT[h * 64:(h + 1) * 64, c * 512:(c + 1) * 512], on, reads=[bon], writes=[C.b_OT])
                    del accs[c]

    nxt = load_head(0)
    for h in range(NH + NMH):
        cur = nxt
        if h + 1 < NH + NMH:
            nxt = load_head(h + 1)
        run_head(h, cur)

def phaseC(C, li, xsrc):
    P, I, NT = C.P, C.I, C.NT
    wo = P.sbuf("w_out", [128, 8, D], BF16)
    b_wo = Buf("w_out")
    P.dma("pool", wo, I["w_out"][li].rearrange("(c p) n -> p c n", p=128), writes=[b_wo])
    otr = Ring(P, "c_ot", 2, [128, 8, 512], BF16)
    xr = Ring(P, "c_x", 3, [128, D], F32)
    banks = [0, 1, 2, 3]
    bi = 0
    for tt in range(NT):
        tok = slice(tt * 128, (tt + 1) * 128)
        if tt % 4 == 0:
            ot, bot = otr.next()
            P.dma("sp", ot, C.OT[:, tt * 128:(tt + 4) * 128].rearrange("(c p) s -> p c s", p=128),
                  reads=[C.b_OT], writes=[bot])
        xt, bx = xr.next()
        P.dma("sp", xt, xsrc[tok, :], reads=[C.b_y[tt]], writes=[bx])
        sub = tt % 4
        for g in range(2):
            ps, bps = C.PS[banks[bi % 4]]
            bi += 1
            for k in range(8):
                P.mm(ps, ot[:, k, sub * 128:(sub + 1) * 128], wo[:, k, g * 512:(g + 1) * 512], k == 0, k == 7,
                     reads=[bot, b_wo], writes=[bps])
            P.tt("dve", xt[:, g * 512:(g + 1) * 512], xt[:, g * 512:(g + 1) * 512], ps, ALU.add,
                 reads=[bx, bps], writes=[bx])
        P.dma("pool", C.y[tok, :], xt, reads=[bx], writes=[C.b_y[tt]])


def phaseD(C, li):
    P, I, NT, S = C.P, C.I, C.NT, C.S
    CH = 256
    NCH = S // CH
    wu = P.sbuf("w_up", [128, 8, 2 * DFF], BF16)
    b_wu = Buf("w_up")
    for k in range(8):
        P.dma("pool", wu[:, k, :], I["ffn_w_up"][li][k * 128:(k + 1) * 128, :], writes=[b_wu])
    wd = P.sbuf("w_dn", [128, NJ, D], BF16)
    b_wd = Buf("w_dn")
    P.dma("pool", wd, I["ffn_w_down"][li].rearrange("(c p) n -> p c n", p=128), writes=[b_wd])
    gf, b_gf = load_gain(C, "g_ffn", I["norm_ffn"][li], D)
    craw = P.sbuf("craw", [44, 4, 128], F32)
    b_craw = Buf("craw")
    for w in range(3):
        P.dma("sp", craw[:, w, :], I["ffn_conv_w"][li, w].rearrange("(c p) -> c p", p=128), writes=[b_craw])
    P.dma("sp", craw[:, 3, :], I["ffn_conv_b"][li].rearrange("(c p) -> c p", p=128), writes=[b_craw])
    cw = P.sbuf("cw", [128, 4, 44], F32)
    b_cw = Buf("cw")
    ps, bps = C.PS[7]
    for w in range(4):
        P.mm(ps[:, w * 64:w * 64 + 44], craw[:, w, :], C.identf[0:44, 0:44], True, True, reads=[b_craw, C.bK], writes=[bps])
    P.copy("dve", cw, ps[:, 0:256].rearrange("p (w c) -> p w c", c=64)[:, :, 0:44], reads=[bps], writes=[b_cw])
    car = P.sbuf("car", [128, 44, 2], F32)
    b_car = Buf("car")
    P.memset("dve", car, 0.0, writes=[b_car])
    xr = Ring(P, "d_x", 3, [128, D], F32)
    jr = Ring(P, "d_junk", 1, [128, D], F32)
    sr = Ring(P, "d_st", 2, [128, 4], F32)
    hr = Ring(P, "d_h", 2, [128, D], BF16)
    h2r = Ring(P, "d_h2T", 1, [128, 8, CH], BF16)
    atr = Ring(P, "d_at", 1, [128, NJ, CH], BF16)
    ugr = Ring(P, "d_ug", 2, [128, CH + 2], F32)
    uvr = Ring(P, "d_uv", 2, [128, CH + 2], F32)
    cgr = Ring(P, "d_cg", 2, [128, 2, CH], F32)
    cvr = Ring(P, "d_cv", 2, [128, 2, CH], F32)
    gb_banks = [1, 2]
    vb_banks = [3, 4]
    dn_banks = [5, 6]
    di = 0
    for ch in range(NCH):
        h2T, bh2 = h2r.next()
        for s2 in range(CH // 128):
            tt = ch * (CH // 128) + s2
            tok = slice(tt * 128, (tt + 1) * 128)
            xt, bx = xr.next()
            junk, bj = jr.next()
            st, bst = sr.next()
            hb, bh = hr.next()
            P.dma("sp", xt, C.y[tok, :], reads=[C.b_y[tt]], writes=[bx])
            rms_rows(C, xt, bx, D, gf, b_gf, hb, bh, junk, bj, st, bst)
            transpose_to(C, hb, bh, 8, 0, h2T[:, :, s2 * 128:(s2 + 1) * 128], bh2)
        at, bat = atr.next()
        for j in range(NJ):
            pg, bpg = C.PS[gb_banks[j % 2]]
            pv, bpv = C.PS[vb_banks[j % 2]]
            for k in range(8):
                P.mm(pg[:, 0:CH], wu[:, k, j * 128:(j + 1) * 128], h2T[:, k, :], k == 0, k == 7,
                     reads=[b_wu, bh2], writes=[bpg])
            for k in range(8):
                P.mm(pv[:, 0:CH], wu[:, k, DFF + j * 128:DFF + (j + 1) * 128], h2T[:, k, :], k == 0, k == 7,
                     reads=[b_wu, bh2], writes=[bpv])
            res = []
            for (pp, bpp, ring, cring, jj) in ((pg, bpg, ugr, cgr, j), (pv, bpv, uvr, cvr, NJ + j)):
                u, bu = ring.next()
                cc, bcc = cring.next()
                P.copy("pool", u[:, 0:2], car[:, jj, :], reads=[b_car], writes=[bu])
                P.copy("act", u[:, 2:CH + 2], pp[:, 0:CH], reads=[bpp], writes=[bu])
                P.copy("pool", car[:, jj, :], u[:, CH:CH + 2], reads=[bu], writes=[b_car])
                P.act(cc[:, 0, :], u[:, 2:CH + 2], AF.Identity, scale=cw[:, 2, jj:jj + 1], bias=cw[:, 3, jj:jj + 1],
                      reads=[bu, b_cw], writes=[bcc])
                P.stt(cc[:, 1, :], u[:, 1:CH + 1], cw[:, 1, jj:jj + 1], cc[:, 0, :], ALU.mult, ALU.add,
                      reads=[bu, b_cw, bcc], writes=[bcc])
                P.stt(cc[:, 0, :], u[:, 0:CH], cw[:, 0, jj:jj + 1], cc[:, 1, :], ALU.mult, ALU.add,
                      reads=[bu, b_cw, bcc], writes=[bcc])
                res.append((cc, bcc))
            (cg, bcg), (cv, bcv) = res
            P.act(cg[:, 1, :], cg[:, 0, :], AF.Silu, reads=[bcg], writes=[bcg])
            P.tt("dve", at[:, j, :], cg[:, 1, :], cv[:, 0, :], ALU.mult, reads=[bcg, bcv], writes=[bat])
        for s2 in range(CH // 128):
            tt = ch * (CH // 128) + s2
            tok = slice(tt * 128, (tt + 1) * 128)
            xt, bx = xr.next()
            ot, bo = xt, bx
            P.dma("sp", xt, C.y[tok, :], reads=[C.b_y[tt]], writes=[bx])
            for g in range(2):
                pd, bpd = C.PS[dn_banks[di % 2]]
                di += 1
                for j in range(NJ):
                    P.mm(pd, at[:, j, s2 * 128:(s2 + 1) * 128], wd[:, j, g * 512:(g + 1) * 512], j == 0, j == NJ - 1,
                         reads=[bat, b_wd], writes=[bpd])
                P.tt("dve", ot[:, g * 512:(g + 1) * 512], xt[:, g * 512:(g + 1) * 512], pd, ALU.add,
                     reads=[bx, bpd], writes=[bo])
            P.dma("sp", C.y[tok, :], ot, reads=[bo], writes=[C.b_y[tt]])


def host_consts(S):
    import ml_dtypes
    bf = ml_dtypes.bfloat16
    idx = np.arange(128)
    c = {}
    c["c_ident"] = np.eye(128, dtype=np.float32).astype(bf)
    c["c_identf"] = np.eye(128, dtype=np.float32)
    c["c_tri"] = (idx[:, None] <= idx[None, :]).astype(np.float32)
    c["c_maskneg"] = np.where(idx[:, None] > idx[None, :], NEG, 0.0).astype(np.float32).astype(bf)
    sel = np.zeros((19, 16, 128), np.float32)
    sel[0:3] = 1.0
    for n in range(16):
        sel[3 + n, n, :] = 1.0
    c["c_sel"] = sel.reshape(19, 16 * 128).astype(bf)
    selS = np.zeros((19, S), np.float32)
    selS[0:3] = 1.0
    blk = np.arange(S) // 256
    for n in range(16):
        selS[3 + n, blk == n] = 1.0
    c["c_selS"] = selS.astype(bf)
    c["c_invfreq"] = (np.float32(10000.0) ** (-(np.arange(0, 32, 2, dtype=np.float32)) / np.float32(32))).astype(np.float32)
    c["c_slopes"] = (2.0 ** (-8.0 * np.arange(1, NH + 1, dtype=np.float32) / NH)).astype(np.float32)
    return c


_WEIGHTS = ["norm_mix", "norm_ffn", "w_out", "mem_norm", "w_mem_kv", "mem_q_gain", "mem_k_gain", "fox_w_in",
            "fox_b_f", "fox_q_gain", "fox_k_gain", "mla_w_in", "mla_qa_norm", "mla_kva_norm", "mla_w_q_up",
            "mla_w_kv_up", "mla_q_gain", "mla_k_gain", "moba_w_in", "moba_q_gain", "moba_k_gain", "ffn_w_up",
            "ffn_conv_w", "ffn_conv_b", "ffn_w_down"]


def make_in_maps(inputs, cores, S):
    consts = host_consts(S)
    shared = {k: np.ascontiguousarray(np.asarray(inputs[k])) for k in _WEIGHTS}
    maps = []
    for b in cores:
        m = dict(shared)
        m.update(consts)
        m["x"] = np.ascontiguousarray(np.asarray(inputs["x"])[b])
        m["mem"] = np.ascontiguousarray(np.asarray(inputs["mem"])[b])
        m["positions"] = np.ascontiguousarray(np.asarray(inputs["positions"])[b].reshape(S, 1).astype(np.int32))
        maps.append(m)
    return maps


def kernel(**inputs):
    B, S, _ = inputs["x"].shape
    nc = build_program(S, [0, 1, 2, 3])
    maps = make_in_maps(inputs, list(range(B)), S)
    res = run_bass_kernel_spmd(nc, maps, core_ids=list(range(B)))
    return np.stack([np.asarray(r["y"]) for r in res.results], axis=0).astype(np.float32)
```

```python
import math
import numpy as np
import concourse.bass as bass
import concourse.mybir as mybir
from concourse.bass_utils import run_bass_kernel_spmd

F32 = mybir.dt.float32
BF16 = mybir.dt.bfloat16
I32 = mybir.dt.int32
ALU = mybir.AluOpType
AF = mybir.ActivationFunctionType
AX = mybir.AxisListType


class Buf:
    __slots__ = ("name", "w", "r")

    def __init__(self, name):
        self.name = name
        self.w = None
        self.r = {}


class Prog:
    CE = ("act", "pool", "pe", "dve")

    def __init__(self, nc, nslots=None):
        self.nc = nc
        nslots = nslots or {"sp": 12, "act": 4, "pool": 8}
        self.streams = {e: [] for e in ("sp", "act", "pool", "pe", "dve")}
        self.slots = {q: [0] * n for q, n in nslots.items()}
        self.slot_ptr = {q: 0 for q in nslots}
        self.sb_off = 0
        self.sb_hi = 0
        self.n_alloc = 0
        self.sb_words = 52992
        self.big = nc.alloc_sbuf_tensor("bigsb", [128, self.sb_words], F32)

    def sbuf(self, name, shape, dtype, align=64):
        esz = {F32: 4, BF16: 2, I32: 4}[dtype]
        nel = int(np.prod(shape[1:]))
        nbytes = (nel * esz + 3) // 4 * 4
        off = (self.sb_off + align - 1) // align * align
        assert off + nbytes <= self.sb_words * 4, f"SBUF overflow allocating {name}: {off + nbytes}"
        self.sb_off = off + nbytes
        self.sb_hi = max(self.sb_hi, self.sb_off)
        v = self.big[0:shape[0], off // 4: off // 4 + nbytes // 4]
        if dtype != F32:
            v = v.bitcast(dtype)
            v = v[:, 0:nel]
        if len(shape) == 3:
            v = v.rearrange("p (a b) -> p a b", b=shape[2])
        elif len(shape) == 4:
            v = v.rearrange("p (a b c) -> p a b c", b=shape[2], c=shape[3])
        return v

    def mark(self):
        return self.sb_off

    def release(self, m):
        self.sb_off = m

    def _deps(self, eng, reads, writes):
        toks = []
        for b in reads:
            if b.w is not None:
                toks.append(b.w)
        for b in writes:
            if b.w is not None:
                toks.append(b.w)
            toks.extend(b.r.values())
        out = []
        for t in toks:
            if t[0] == "c":
                if t[1] == eng and eng == "pe":
                    continue
                self.streams[t[1]][t[2]]["sig"] = True
            out.append(t)
        return out

    def _commit(self, tok, reads, writes):
        key = tok[1]
        for b in writes:
            b.w = tok
            b.r = {}
        for b in reads:
            if b in writes:
                continue
            b.r[key] = tok

    def op(self, eng, fn, reads=(), writes=()):
        st = self.streams[eng]
        idx = len(st)
        waits = self._deps(eng, reads, writes)
        st.append(dict(fn=fn, waits=waits, sig=False, dma=None))
        tok = ("c", eng, idx)
        self._commit(tok, reads, writes)
        return tok

    def dma(self, q, out, in_, reads=(), writes=(), **kw):
        st = self.streams[q]
        waits = self._deps(q, reads, writes)
        n = len(self.slots[q])
        s = self.slot_ptr[q]
        self.slot_ptr[q] = (s + 1) % n
        prev = self.slots[q][s]
        if prev > 0:
            waits.append(("d", (q, s), prev))
        self.slots[q][s] = prev + 16
        tok = ("d", (q, s), prev + 16)
        st.append(dict(fn=lambda e: e.dma_start(out=out, in_=in_, **kw), waits=waits, sig=False, dma=(q, s)))
        self._commit(tok, reads, writes)
        return tok


    def act(self, out, in_, func, reads=(), writes=(), **kw):
        return self.op("act", lambda e: e.activation(out=out, in_=in_, func=func, **kw), reads, writes)

    def tt(self, eng, out, in0, in1, op, reads=(), writes=()):
        return self.op(eng, lambda e: e.tensor_tensor(out=out, in0=in0, in1=in1, op=op), reads, writes)

    def ts(self, eng, out, in0, s1, s2, op0, op1=None, reads=(), writes=()):
        if op1 is None:
            return self.op(eng, lambda e: e.tensor_scalar(out=out, in0=in0, scalar1=s1, scalar2=None, op0=op0), reads, writes)
        return self.op(eng, lambda e: e.tensor_scalar(out=out, in0=in0, scalar1=s1, scalar2=s2, op0=op0, op1=op1), reads, writes)

    def stt(self, out, in0, scalar, in1, op0, op1, reads=(), writes=()):
        return self.op("dve", lambda e: e.scalar_tensor_tensor(out=out, in0=in0, scalar=scalar, in1=in1, op0=op0, op1=op1), reads, writes)

    def copy(self, eng, out, in_, reads=(), writes=()):
        if eng == "act":
            return self.op("act", lambda e: e.copy(out=out, in_=in_), reads, writes)
        return self.op(eng, lambda e: e.tensor_copy(out=out, in_=in_), reads, writes)

    def reduce(self, eng, out, in_, op, reads=(), writes=(), axis=None):
        ax = axis or AX.X
        return self.op(eng, lambda e: e.tensor_reduce(out=out, in_=in_, axis=ax, op=op), reads, writes)

    def mm(self, out, lhsT, rhs, start, stop, reads=(), writes=()):
        return self.op("pe", lambda e: e.matmul(out, lhsT=lhsT, rhs=rhs, start=start, stop=stop), reads, writes)

    def transpose(self, out, in_, ident, reads=(), writes=()):
        return self.op("pe", lambda e: e.transpose(out, in_, ident), reads, writes)

    def memset(self, eng, ap, val, writes=()):
        return self.op(eng, lambda e: e.memset(ap, val), (), writes)

    def recip(self, out, in_, reads=(), writes=()):
        return self.op("dve", lambda e: e.reciprocal(out=out, in_=in_), reads, writes)

    def max8(self, out, in_, reads=(), writes=()):
        return self.op("dve", lambda e: e.max(out=out, in_=in_), reads, writes)

    def barrier(self):
        toks = []
        for E in self.CE:
            st = self.streams[E]
            for i in range(len(st) - 1, -1, -1):
                if st[i]["dma"] is None and st[i]["fn"] is not None:
                    st[i]["sig"] = True
                    toks.append(("c", E, i))
                    break
        for q, vals in self.slots.items():
            for s, v in enumerate(vals):
                if v > 0:
                    toks.append(("d", (q, s), v))
        for E in self.streams:
            self.streams[E].append(dict(fn=None, waits=list(toks), sig=False, dma=None))

    def finalize(self):
        nc = self.nc
        self.barrier()
        cnt = {}
        for E in self.CE:
            c = 0
            arr = []
            for ent in self.streams[E]:
                if ent["sig"]:
                    c += 1
                arr.append(c)
            cnt[E] = arr
        sems = {}
        for E in self.CE:
            sems[("c", E)] = nc.alloc_semaphore(f"s_{E}")
        for q, vals in self.slots.items():
            for s in range(len(vals)):
                sems[("d", (q, s))] = nc.alloc_semaphore(f"d_{q}{s}")

        def run(E):
            def body(e):
                waited = {}
                for ent in self.streams[E]:
                    for t in ent["waits"]:
                        if t[0] == "c":
                            key = ("c", t[1])
                            val = cnt[t[1]][t[2]]
                        else:
                            key = ("d", t[1])
                            val = t[2]
                        if waited.get(key, 0) >= val:
                            continue
                        waited[key] = val
                        e.wait_ge(sems[key], val)
                    if ent["fn"] is None:
                        continue
                    ins = ent["fn"](e)
                    if ent["dma"] is not None:
                        ins.then_inc(sems[("d", ent["dma"])], 16)
                    elif ent["sig"]:
                        ins.then_inc(sems[("c", E)], 1)
            return body

        with nc.Block() as block:
            block.sync(run("sp"))
            block.scalar(run("act"))
            block.gpsimd(run("pool"))
            block.tensor(run("pe"))
            block.vector(run("dve"))
        return nc

D = 1024
NH = 12
HD = 64
NMH = 4
MEM = 256
DFF = 2816
NJ = DFF // 128
EPS = 1e-6
FOX_IN = 3 * 768 + 12 + 256
MLA_IN = 256 + 128 + 32 + 256
MOBA_IN = 3 * 768 + 256
NEG = -32768.0
TWO_PI = 6.283185307179586
CW1 = 6.28125
CW2 = TWO_PI - CW1


class Ring:
    def __init__(self, P, name, n, shape, dtype):
        self.items = [(P.sbuf(f"{name}{i}", shape, dtype), Buf(f"{name}{i}")) for i in range(n)]
        self.i = 0

    def next(self):
        it = self.items[self.i % len(self.items)]
        self.i += 1
        return it


def bc_mid(ap, n):
    return ap.unsqueeze(1).broadcast_to([ap.shape[0], n, ap.shape[1]])


def bc_last(ap, n):
    return ap.unsqueeze(2).broadcast_to([ap.shape[0], ap.shape[1], n])


def col_groups(n, w=512):
    return [(c, min(c + w, n)) for c in range(0, n, w)]


class Ctx:
    pass


def build_program(S, layer_ids, dbg=False):
    NT = S // 128
    nc = bass.Bass("TRN2", target_bir_lowering=False)
    P = Prog(nc)
    C = Ctx()
    C.S, C.NT, C.nc, C.P = S, NT, nc, P

    def din(name, shape, dt=F32):
        return nc.dram_tensor(name, list(shape), dt, kind="ExternalInput").ap()

    I = {}
    I["x"] = din("x", [S, D])
    I["mem"] = din("mem", [MEM, D])
    I["positions"] = din("positions", [S, 1], I32)
    I["norm_mix"] = din("norm_mix", [4, D])
    I["norm_ffn"] = din("norm_ffn", [4, D])
    I["w_out"] = din("w_out", [4, D, D])
    I["mem_norm"] = din("mem_norm", [D])
    I["w_mem_kv"] = din("w_mem_kv", [D, 512])
    I["mem_q_gain"] = din("mem_q_gain", [4, HD])
    I["mem_k_gain"] = din("mem_k_gain", [4, HD])
    I["fox_w_in"] = din("fox_w_in", [2, D, FOX_IN])
    I["fox_b_f"] = din("fox_b_f", [2, NH])
    I["fox_q_gain"] = din("fox_q_gain", [2, HD])
    I["fox_k_gain"] = din("fox_k_gain", [2, HD])
    I["mla_w_in"] = din("mla_w_in", [1, D, MLA_IN])
    I["mla_qa_norm"] = din("mla_qa_norm", [1, 256])
    I["mla_kva_norm"] = din("mla_kva_norm", [1, 128])
    I["mla_w_q_up"] = din("mla_w_q_up", [1, 256, NH * 96])
    I["mla_w_kv_up"] = din("mla_w_kv_up", [1, 128, NH * 128])
    I["mla_q_gain"] = din("mla_q_gain", [1, 96])
    I["mla_k_gain"] = din("mla_k_gain", [1, 96])
    I["moba_w_in"] = din("moba_w_in", [1, D, MOBA_IN])
    I["moba_q_gain"] = din("moba_q_gain", [1, HD])
    I["moba_k_gain"] = din("moba_k_gain", [1, HD])
    I["ffn_w_up"] = din("ffn_w_up", [4, D, 2 * DFF])
    I["ffn_conv_w"] = din("ffn_conv_w", [4, 3, 2 * DFF])
    I["ffn_conv_b"] = din("ffn_conv_b", [4, 2 * DFF])
    I["ffn_w_down"] = din("ffn_w_down", [4, DFF, D])
    I["c_ident"] = din("c_ident", [128, 128], BF16)
    I["c_identf"] = din("c_identf", [128, 128])
    I["c_tri"] = din("c_tri", [128, 128])
    I["c_maskneg"] = din("c_maskneg", [128, 128], BF16)
    I["c_sel"] = din("c_sel", [19, 16 * 128], BF16)
    I["c_selS"] = din("c_selS", [19, S], BF16)
    I["c_invfreq"] = din("c_invfreq", [16])
    I["c_slopes"] = din("c_slopes", [NH])
    C.I = I
    y = nc.dram_tensor("y", [S, D], F32, kind="ExternalOutput").ap()
    C.y = y
    C.b_y = [Buf(f"y{t}") for t in range(NT)]
    C.QT = nc.dram_tensor("QT", [NH * 96, S], BF16).ap()
    C.KT = nc.dram_tensor("KT", [NH * 96, S], BF16).ap()
    C.QmT = nc.dram_tensor("QmT", [NMH * HD, S], BF16).ap()
    C.V = nc.dram_tensor("Vs", [S, NH * HD], BF16).ap()
    C.AUX = nc.dram_tensor("AUX", [NH, 19, S], BF16).ap()
    C.OT = nc.dram_tensor("OT", [D, S], BF16).ap()
    C.b_QT, C.b_KT, C.b_QmT, C.b_V, C.b_AUX, C.b_OT = (Buf(n) for n in ("QT", "KT", "QmT", "V", "AUX", "OT"))
    C.dbg = {}
    C.PS = []
    for i in range(8):
        t = nc.alloc_psum_tensor(f"psb{i}", [128, 512], F32)
        C.PS.append((t[:], Buf(f"ps{i}")))
    C.bK = Buf("consts")
    C.ident = P.sbuf("ident", [128, 128], BF16)
    C.identf = P.sbuf("identf", [128, 128], F32)
    C.tri = P.sbuf("tri", [128, 128], F32)
    C.maskneg = P.sbuf("maskneg", [128, 128], BF16)
    C.sel = P.sbuf("sel", [19, 16 * 128], BF16)
    C.ones_bf = P.sbuf("ones_bf", [128, 128], BF16)
    C.ones_f = P.sbuf("ones_f", [128, 128], F32)
    C.invf = P.sbuf("invf", [128, 16], F32)
    C.slopes = P.sbuf("slopes", [128, NH], F32)
    P.dma("sp", C.ident, I["c_ident"], writes=[C.bK])
    P.dma("sp", C.identf, I["c_identf"], writes=[C.bK])
    P.dma("sp", C.tri, I["c_tri"], writes=[C.bK])
    P.dma("sp", C.maskneg, I["c_maskneg"], writes=[C.bK])
    P.dma("sp", C.sel, I["c_sel"], writes=[C.bK])
    P.dma("sp", C.invf, I["c_invfreq"].partition_broadcast(128), writes=[C.bK])
    P.dma("sp", C.slopes, I["c_slopes"].partition_broadcast(128), writes=[C.bK])
    P.memset("dve", C.ones_bf, 1.0, writes=[C.bK])
    P.memset("dve", C.ones_f, 1.0, writes=[C.bK])
    C.kmem_raw = P.sbuf("kmem_raw", [128, 2, 256], F32)
    C.b_kmem = Buf("kmem_raw")
    C.vmaug = P.sbuf("vmaug", [128, 2, NMH, 128], BF16)
    C.b_vmaug = Buf("vmaug")
    C.posf = P.sbuf("posf", [128, NT], F32)
    C.b_posf = Buf("posf")
    C.cos = P.sbuf("cos", [128, NT, 16], F32)
    C.sin = P.sbuf("sin", [128, NT, 16], F32)
    C.b_cs = Buf("cossin")
    C.bias_tab = P.sbuf("bias_tab", [128, NT, NH], F32)
    C.b_bias = Buf("bias_tab")
    C.kmT = P.sbuf("kmT", [64, NMH, MEM], BF16)
    C.b_kmT = Buf("kmT")
    P.barrier()
    phase0(C)
    import os as _os
    _stop = _os.environ.get("KSTOP", "Z")
    for li in layer_ids:
        if _stop == "0":
            break
        kind, j = li % 3, li // 3
        xsrc = I["x"] if li == layer_ids[0] else y
        P.barrier()
        m = P.mark()
        phaseA(C, li, kind, j, xsrc)
        P.release(m)
        if _stop == "A":
            break
        P.barrier()
        m = P.mark()
        phaseB(C, kind)
        P.release(m)
        if _stop == "B":
            break
        P.barrier()
        m = P.mark()
        phaseC(C, li, xsrc)
        P.release(m)
        if _stop == "C":
            break
        P.barrier()
        m = P.mark()
        phaseD(C, li)
        P.release(m)
    P.finalize()
    return nc

def rms_rows(C, xt, bx, W, gb, bg, out, bout, junk, bjunk, st, bst):
    P = C.P
    P.act(junk, xt, AF.Square, reads=[bx], writes=[bjunk])
    P.reduce("dve", st[:, 0:1], junk, ALU.add, reads=[bjunk], writes=[bst])
    P.ts("dve", st[:, 1:2], st[:, 0:1], 1.0 / W, EPS, ALU.mult, ALU.add, reads=[bst], writes=[bst])
    P.act(st[:, 2:3], st[:, 1:2], AF.Ln, reads=[bst], writes=[bst])
    P.act(st[:, 3:4], st[:, 2:3], AF.Exp, scale=-0.5, reads=[bst], writes=[bst])
    P.stt(out, xt, st[:, 3:4], gb, ALU.mult, ALU.mult, reads=[bx, bst, bg], writes=[bout])


def head_norm(C, eng, src3, bsrc, nh, hd, gain3, bgain, out3, bout, junk3, bjunk, st, bst):
    P = C.P
    P.act(junk3, src3, AF.Square, reads=[bsrc], writes=[bjunk])
    P.reduce("dve", st[:, 0, 0:nh], junk3, ALU.add, reads=[bjunk], writes=[bst])
    P.ts("dve", st[:, 1, 0:nh], st[:, 0, 0:nh], 1.0 / hd, EPS, ALU.mult, ALU.add, reads=[bst], writes=[bst])
    P.act(st[:, 2, 0:nh], st[:, 1, 0:nh], AF.Ln, reads=[bst], writes=[bst])
    P.act(st[:, 3, 0:nh], st[:, 2, 0:nh], AF.Exp, scale=-0.5, reads=[bst], writes=[bst])
    P.tt(eng, junk3, src3, bc_last(st[:, 3, 0:nh], hd), ALU.mult, reads=[bsrc, bst], writes=[bjunk])
    P.tt(eng, out3, junk3, gain3, ALU.mult, reads=[bjunk, bgain], writes=[bout])


def transpose_to(C, src_bf, bsrc, nblk, bank, dst, bdst, rows=128, blkw=128, evac="act"):
    P = C.P
    ps, bps = C.PS[bank]
    psb = ps.bitcast(BF16)
    done = 0
    while done < nblk:
        n = min(8, nblk - done)
        for b in range(n):
            P.transpose(psb[0:blkw, b * 128:(b + 1) * 128], src_bf[:, (done + b) * blkw:(done + b + 1) * blkw],
                        C.ident, reads=[bsrc, C.bK], writes=[bps])
        P.copy(evac, dst[0:blkw, done:done + n, :], psb[0:blkw, 0:n * 128].rearrange("p (a b) -> p a b", b=128),
               reads=[bps], writes=[bdst])
        done += n


def phase0(C):
    P, I, NT = C.P, C.I, C.NT
    import os as _os
    _p0 = _os.environ.get("KP0", "z")
    if _p0 == "a":
        return
    m = P.mark()
    posi = P.sbuf("posi", [128, NT, 1], I32)
    b_posi = Buf("posi")
    P.dma("sp", posi, I["positions"].rearrange("(t p) o -> p t o", p=128), writes=[b_posi], allow_slow_non_contiguous=True)
    P.copy("dve", C.posf, posi[:, :, 0], reads=[b_posi], writes=[C.b_posf])
    if _p0 == "b":
        return
    ang = P.sbuf("ang", [128, NT, 16], F32)
    b_ang = Buf("ang")
    tmp = P.sbuf("rtmp", [128, NT, 16], F32)
    b_tmp = Buf("rtmp")
    tmp2 = P.sbuf("rtmp2", [128, NT, 16], F32)
    b_tmp2 = Buf("rtmp2")
    ki = P.sbuf("rki", [128, NT, 16], I32)
    b_ki = Buf("rki")
    P.tt("dve", ang, bc_last(C.posf, 16), bc_mid(C.invf, NT), ALU.mult, reads=[C.b_posf, C.bK], writes=[b_ang])
    for which, dst in ((0, C.sin), (1, C.cos)):
        src = ang
        bsrc = b_ang
        if which == 1:
            P.ts("dve", tmp2, ang, math.pi / 2, None, ALU.add, reads=[b_ang], writes=[b_tmp2])
            src, bsrc = tmp2, b_tmp2
        P.ts("dve", tmp, src, 1.0 / TWO_PI, None, ALU.mult, reads=[bsrc], writes=[b_tmp])
        P.copy("dve", ki, tmp, reads=[b_tmp], writes=[b_ki])
        P.copy("dve", tmp, ki, reads=[b_ki], writes=[b_tmp])
        P.stt(dst, tmp, -CW1, src, ALU.mult, ALU.add, reads=[b_tmp, bsrc], writes=[C.b_cs])
        P.stt(dst, tmp, -CW2, dst, ALU.mult, ALU.add, reads=[b_tmp, C.b_cs], writes=[C.b_cs])
        P.ts("dve", dst, dst, math.pi, -math.pi, ALU.min, ALU.max, reads=[C.b_cs], writes=[C.b_cs])
        P.act(dst, dst, AF.Sin, reads=[C.b_cs], writes=[C.b_cs])
    if _p0 == "c":
        return
    wkv = P.sbuf("wkv", [128, 8, 512], BF16)
    b_wkv = Buf("wkv")
    for k in range(8):
        P.dma("pool", wkv[:, k, :], I["w_mem_kv"][k * 128:(k + 1) * 128, :], writes=[b_wkv])
    gb = P.sbuf("g_mem", [128, D], F32)
    b_gb = Buf("g_mem")
    P.dma("sp", gb, I["mem_norm"].partition_broadcast(128), writes=[b_gb])
    P.memset("dve", C.vmaug, 1.0, writes=[C.b_vmaug])
    xr = Ring(P, "m_x", 2, [128, D], F32)
    jr = Ring(P, "m_junk", 2, [128, D], F32)
    sr = Ring(P, "m_st", 2, [128, 4], F32)
    hr = Ring(P, "m_h", 2, [128, D], BF16)
    tr = Ring(P, "m_hT", 2, [128, 8, 128], BF16)
    for mt in range(2):
        if _p0 == "d1":
            break
        xt, bx = xr.next()
        junk, bj = jr.next()
        st, bst = sr.next()
        hb, bh = hr.next()
        hT, bhT = tr.next()
        P.dma("sp", xt, I["mem"][mt * 128:(mt + 1) * 128, :], writes=[bx])
        rms_rows(C, xt, bx, D, gb, b_gb, hb, bh, junk, bj, st, bst)
        if _p0 == "d2":
            break
        transpose_to(C, hb, bh, 8, mt, hT, bhT)
        if _p0 == "d3":
            break
        ps, bps = C.PS[2 + mt]
        for k in range(8):
            P.mm(ps, hT[:, k, :], wkv[:, k, :], k == 0, k == 7, reads=[bhT, b_wkv], writes=[bps])
        if _p0 == "d4":
            break
        P.copy("act", C.kmem_raw[:, mt, :], ps[:, 0:256], reads=[bps], writes=[C.b_kmem])
        if _p0 == "d5":
            continue
        P.copy("act", C.vmaug[:, mt, :, 0:64], ps[:, 256:512].rearrange("p (h d) -> p h d", d=64),
               reads=[bps], writes=[C.b_vmaug])
    P.release(m)

def load_gain(C, name, src_row, W, scale=None):
    P = C.P
    t = P.sbuf(name, [128, W], F32)
    b = Buf(name)
    P.dma("sp", t, src_row.partition_broadcast(128), writes=[b])
    if scale is not None:
        P.ts("dve", t, t, float(scale), None, ALU.mult, reads=[b], writes=[b])
    return t, b


def phaseA(C, li, kind, j, xsrc):
    P, I, NT, S = C.P, C.I, C.NT, C.S
    if kind == 0:
        w_in, NIN = I["fox_w_in"][j], FOX_IN
        q_gain, k_gain = I["fox_q_gain"][j], I["fox_k_gain"][j]
        QM0 = 2316
    elif kind == 1:
        w_in, NIN = I["mla_w_in"][j], MLA_IN
        QM0 = 416
    else:
        w_in, NIN = I["moba_w_in"][j], MOBA_IN
        q_gain, k_gain = I["moba_q_gain"][j], I["moba_k_gain"][j]
        QM0 = 2304
    DK = 96 if kind == 1 else 64
    wbf = P.sbuf("w_in", [128, 8, NIN], BF16)
    b_w = Buf("w_in")
    for k in range(8):
        P.dma("pool", wbf[:, k, :], w_in[k * 128:(k + 1) * 128, :], writes=[b_w])
    gmix, b_gmix = load_gain(C, "g_mix", I["norm_mix"][li], D)
    gmq, b_gmq = load_gain(C, "g_mq", I["mem_q_gain"][li], HD, HD ** -0.5)
    gmk, b_gmk = load_gain(C, "g_mk", I["mem_k_gain"][li], HD)
    if kind != 1:
        gq, b_gq = load_gain(C, "g_q", q_gain, HD, HD ** -0.5)
        gk, b_gk = load_gain(C, "g_k", k_gain, HD)
    else:
        gq, b_gq = load_gain(C, "g_q", I["mla_q_gain"][j], 96, 96 ** -0.5)
        gk, b_gk = load_gain(C, "g_k", I["mla_k_gain"][j], 96)
        gqa, b_gqa = load_gain(C, "g_qa", I["mla_qa_norm"][j], 256)
        gkva, b_gkva = load_gain(C, "g_kva", I["mla_kva_norm"][j], 128)
        wq = P.sbuf("w_qup", [128, 2, NH * 96], BF16)
        b_wq = Buf("w_qup")
        P.dma("pool", wq, I["mla_w_q_up"][j].rearrange("(c p) n -> p c n", p=128), writes=[b_wq])
        wkv = P.sbuf("w_kvup", [128, NH * 128], BF16)
        b_wkv = Buf("w_kvup")
        P.dma("pool", wkv, I["mla_w_kv_up"][j], writes=[b_wkv])
    if kind == 0:
        bfb, b_bfb = load_gain(C, "b_f", I["fox_b_f"][j], NH)
        carry = P.sbuf("carry", [1, NH], F32)
        b_carry = Buf("carry")
        P.memset("dve", carry, 0.0, writes=[b_carry])
    if kind == 2:
        P.tt("dve", C.bias_tab, bc_last(C.posf, NH), bc_mid(C.slopes, NT), ALU.mult,
             reads=[C.b_posf, C.bK], writes=[C.b_bias])
        ksum = P.sbuf("ksum", [128, 6, NT], F32)
        b_ksum = Buf("ksum")
        kmean = P.sbuf("kmean", [128, 6, 32], BF16)
        b_kmean = Buf("kmean")
        P.memset("dve", kmean, 0.0, writes=[b_kmean])
        nmrows = [P.sbuf("nmrows0", [128, S], BF16), P.sbuf("nmrows1", [64, S], BF16)]
        b_nmrows = Buf("nmrows")
    if kind != 1:
        prow = P.sbuf("prow", [36, S], BF16)
        b_prow = Buf("prow")
    mj = P.sbuf("mk_junk", [128, NMH, HD], F32)
    b_mj = Buf("mk_junk")
    mst = P.sbuf("mk_st", [128, 4, 16], F32)
    b_mst = Buf("mk_st")
    mkn = P.sbuf("mk_n", [128, NMH * HD], BF16)
    b_mkn = Buf("mk_n")
    for mt in range(2):
        head_norm(C, "dve", C.kmem_raw[:, mt, :].rearrange("p (h d) -> p h d", d=HD), C.b_kmem, NMH, HD,
                  bc_mid(gmk, NMH), b_gmk, mkn.rearrange("p (h d) -> p h d", d=HD), b_mkn, mj, b_mj, mst, b_mst)
        ps, bps = C.PS[7]
        psb = ps.bitcast(BF16)
        for h in range(NMH):
            P.transpose(psb[0:64, h * 128:(h + 1) * 128], mkn[:, h * 64:(h + 1) * 64], C.ident,
                        reads=[b_mkn, C.bK], writes=[bps])
        P.copy("act", C.kmT[:, :, mt * 128:(mt + 1) * 128], psb[0:64, 0:512].rearrange("p (h m) -> p h m", m=128),
               reads=[bps], writes=[C.b_kmT])
    xr = Ring(P, "a_x", 2, [128, D], F32)
    jr = Ring(P, "a_junk", 2, [128, D], F32)
    sr = Ring(P, "a_st", 2, [128, 4], F32)
    hr = Ring(P, "a_h", 2, [128, D], BF16)
    tr = Ring(P, "a_hT", 2, [128, 8, 128], BF16)
    pr = Ring(P, "a_p", 2, [128, NIN], F32)
    NQ = NH * DK
    qnr = Ring(P, "a_qn", 2, [128, NQ], BF16)
    knr = Ring(P, "a_kn", 2, [128, NQ], BF16)
    qmr = Ring(P, "a_qm", 2, [128, 256], BF16)
    vbr = Ring(P, "a_vb", 2, [128, 768], BF16)
    hjq = Ring(P, "a_hjq", 1, [128, NQ], F32)
    hjk = Ring(P, "a_hjk", 1, [128, NQ], F32)
    hjm = Ring(P, "a_hjm", 1, [128, 256], F32)
    hsq = Ring(P, "a_hsq", 2, [128, 4, 16], F32)
    hsk = Ring(P, "a_hsk", 2, [128, 4, 16], F32)
    hsm = Ring(P, "a_hsm", 2, [128, 4, 16], F32)
    NBQ = NQ // 128 if kind != 1 else NH
    BW = 128 if kind != 1 else 96
    qts = Ring(P, "a_qts", 2, [128, NBQ, 512], BF16)
    kts = Ring(P, "a_kts", 2, [128, NBQ, 512], BF16)
    qmts = Ring(P, "a_qmts", 2, [128, 2, 512], BF16)
    small = Ring(P, "a_small", 2, [128, 8, 16], F32)
    pcs = Ring(P, "a_pcs", 2, [128, NH * 3], BF16)
    if kind == 1:
        qlr = Ring(P, "a_ql", 2, [128, 256], BF16)
        kvlr = Ring(P, "a_kvl", 2, [128, 128], BF16)
        qlT = Ring(P, "a_qlT", 2, [128, 2, 128], BF16)
        kvT = Ring(P, "a_kvT", 2, [128, 1, 128], BF16)
        qfr = Ring(P, "a_qf", 1, [128, NH, 96], F32)
        kvfr = Ring(P, "a_kvf", 1, [128, NH, 128], F32)
        kfr = Ring(P, "a_kf", 1, [128, NH, 96], F32)
        rtmp = Ring(P, "a_rt", 2, [128, 4, NH, 16], F32)
        krr = Ring(P, "a_kr", 2, [128, 32], F32)
    if kind == 2:
        gater = Ring(P, "a_gate", 2, [128, NH, 16], F32)
        top8 = Ring(P, "a_top8", 2, [128, NH, 8], F32)
        nmr = Ring(P, "a_nm", 2, [128, NH * 16], BF16)
        qtc = Ring(P, "a_qtc", 2, [128, 6, 128], BF16)
    groups = col_groups(NIN)
    gbank = [2, 3, 4]
    gi = 0
    cur_qts = cur_kts = cur_qmts = None
    gstate = dict(gi=0)

    def front(tt):
        tok = slice(tt * 128, (tt + 1) * 128)
        xt, bx = xr.next()
        junk, bj = jr.next()
        st, bst = sr.next()
        hb, bh = hr.next()
        hT, bhT = tr.next()
        psb_, bp = pr.next()
        P.dma("sp", xt, xsrc[tok, :], reads=[C.b_y[tt]], writes=[bx])
        rms_rows(C, xt, bx, D, gmix, b_gmix, hb, bh, junk, bj, st, bst)
        transpose_to(C, hb, bh, 8, tt % 2, hT, bhT)
        for (c0, c1) in groups:
            ps, bps = C.PS[gbank[gstate["gi"] % 3]]
            gstate["gi"] += 1
            for k in range(8):
                P.mm(ps[:, 0:c1 - c0], hT[:, k, :], wbf[:, k, c0:c1], k == 0, k == 7, reads=[bhT, b_w], writes=[bps])
            P.copy("act" if gstate["gi"] % 2 else "dve", psb_[:, c0:c1], ps[:, 0:c1 - c0], reads=[bps], writes=[bp])
        return dict(psb_=psb_, bp=bp, junk=junk, bj=bj, st=st, bst=bst)

    fstate = {0: front(0)}
    for tt in range(NT):
        if tt + 1 < NT:
            fstate[tt + 1] = front(tt + 1)
        F_ = fstate.pop(tt)
        psb_, bp, junk, bj, st, bst = F_["psb_"], F_["bp"], F_["junk"], F_["bj"], F_["st"], F_["bst"]
        gi = gstate["gi"]
        tok = slice(tt * 128, (tt + 1) * 128)
        sub = tt % 4
        if sub == 0:
            cur_qts, cur_kts, cur_qmts = qts.next(), kts.next(), qmts.next()
        (qT, bqT), (kT, bkT), (qmT, bqmT) = cur_qts, cur_kts, cur_qmts
        qmn, bqmn = qmr.next()
        j3, bj3 = hjm.next()
        s3, bs3 = hsm.next()
        head_norm(C, "pool", psb_[:, QM0:QM0 + 256].rearrange("p (h d) -> p h d", d=HD), bp, NMH, HD,
                  bc_mid(gmq, NMH), b_gmq, qmn.rearrange("p (h d) -> p h d", d=HD), bqmn,
                  j3.rearrange("p (h d) -> p h d", d=HD), bj3, s3, bs3)
        if kind != 1:
            qn, bqn = qnr.next()
            kn, bkn = knr.next()
            vb, bvb = vbr.next()
            jq, bjq = hjq.next()
            jk, bjk = hjk.next()
            sq, bsq = hsq.next()
            sk, bsk = hsk.next()
            head_norm(C, "dve", psb_[:, 0:768].rearrange("p (h d) -> p h d", d=HD), bp, NH, HD,
                      bc_mid(gq, NH), b_gq, qn.rearrange("p (h d) -> p h d", d=HD), bqn,
                      jq.rearrange("p (h d) -> p h d", d=HD), bjq, sq, bsq)
            head_norm(C, "pool", psb_[:, 768:1536].rearrange("p (h d) -> p h d", d=HD), bp, NH, HD,
                      bc_mid(gk, NH), b_gk, kn.rearrange("p (h d) -> p h d", d=HD), bkn,
                      jk.rearrange("p (h d) -> p h d", d=HD), bjk, sk, bsk)
            P.copy("act", vb, psb_[:, 1536:2304], reads=[bp], writes=[bvb])
            P.dma("pool", C.V[tok, :], vb, reads=[bvb], writes=[C.b_V])
        else:
            ql, bql = qlr.next()
            kvl, bkvl = kvlr.next()
            rms_rows(C, psb_[:, 0:256], bp, 256, gqa, b_gqa, ql, bql, junk[:, 0:256], bj, st, bst)
            rms_rows(C, psb_[:, 256:384], bp, 128, gkva, b_gkva, kvl, bkvl, junk[:, 256:384], bj, st, bst)
            qlt, bqlt = qlT.next()
            kvt, bkvt = kvT.next()
            transpose_to(C, ql, bql, 2, 7, qlt, bqlt)
            transpose_to(C, kvl, bkvl, 1, 7, kvt, bkvt)
            qf, bqf = qfr.next()
            kvf, bkvf = kvfr.next()
            kf, bkf = kfr.next()
            qf2 = qf.rearrange("p h d -> p (h d)")
            kvf2 = kvf.rearrange("p h d -> p (h d)")
            for (c0, c1) in col_groups(NH * 96):
                ps, bps = C.PS[gbank[gi % 3]]
                gi += 1
                for k in range(2):
                    P.mm(ps[:, 0:c1 - c0], qlt[:, k, :], wq[:, k, c0:c1], k == 0, k == 1, reads=[bqlt, b_wq], writes=[bps])
                P.copy("act" if gi % 2 else "dve", qf2[:, c0:c1], ps[:, 0:c1 - c0], reads=[bps], writes=[bqf])
            for (c0, c1) in col_groups(NH * 128):
                ps, bps = C.PS[gbank[gi % 3]]
                gi += 1
                P.mm(ps[:, 0:c1 - c0], kvt[:, 0, :], wkv[:, c0:c1], True, True, reads=[bkvt, b_wkv], writes=[bps])
                P.copy("act" if gi % 2 else "dve", kvf2[:, c0:c1], ps[:, 0:c1 - c0], reads=[bps], writes=[bkvf])
            gstate["gi"] = gi
            cosb = C.cos[:, tt, :]
            sinb = C.sin[:, tt, :]
            rt, brt = rtmp.next()
            x1 = qf[:, :, 64:80]
            x2 = qf[:, :, 80:96]
            P.tt("dve", rt[:, 0], x1, bc_mid(cosb, NH), ALU.mult, reads=[bqf, C.b_cs], writes=[brt])
            P.tt("dve", rt[:, 1], x2, bc_mid(sinb, NH), ALU.mult, reads=[bqf, C.b_cs], writes=[brt])
            P.tt("dve", rt[:, 2], x1, bc_mid(sinb, NH), ALU.mult, reads=[bqf, C.b_cs], writes=[brt])
            P.tt("dve", rt[:, 3], x2, bc_mid(cosb, NH), ALU.mult, reads=[bqf, C.b_cs], writes=[brt])
            P.tt("dve", x1, rt[:, 0], rt[:, 1], ALU.subtract, reads=[brt], writes=[bqf])
            P.tt("dve", x2, rt[:, 2], rt[:, 3], ALU.add, reads=[brt], writes=[bqf])
            kr, bkr = krr.next()
            k1 = psb_[:, 384:400]
            k2 = psb_[:, 400:416]
            P.tt("pool", rt[:, 0, 0], k1, cosb, ALU.mult, reads=[bp, C.b_cs, brt], writes=[brt])
            P.tt("pool", rt[:, 1, 0], k2, sinb, ALU.mult, reads=[bp, C.b_cs], writes=[brt])
            P.tt("pool", rt[:, 2, 0], k1, sinb, ALU.mult, reads=[bp, C.b_cs], writes=[brt])
            P.tt("pool", rt[:, 3, 0], k2, cosb, ALU.mult, reads=[bp, C.b_cs], writes=[brt])
            P.tt("pool", kr[:, 0:16], rt[:, 0, 0], rt[:, 1, 0], ALU.subtract, reads=[brt], writes=[bkr])
            P.tt("pool", kr[:, 16:32], rt[:, 2, 0], rt[:, 3, 0], ALU.add, reads=[brt], writes=[bkr])
            P.copy("pool", kf[:, :, 0:64], kvf[:, :, 0:64], reads=[bkvf], writes=[bkf])
            P.copy("pool", kf[:, :, 64:96], bc_mid(kr, NH), reads=[bkr], writes=[bkf])
            qn, bqn = qnr.next()
            kn, bkn = knr.next()
            vb, bvb = vbr.next()
            jq, bjq = hjq.next()
            jk, bjk = hjk.next()
            sq, bsq = hsq.next()
            sk, bsk = hsk.next()
            head_norm(C, "dve", qf, bqf, NH, 96, bc_mid(gq, NH), b_gq, qn.rearrange("p (h d) -> p h d", d=96), bqn,
                      jq.rearrange("p (h d) -> p h d", d=96), bjq, sq, bsq)
            head_norm(C, "pool", kf, bkf, NH, 96, bc_mid(gk, NH), b_gk, kn.rearrange("p (h d) -> p h d", d=96), bkn,
                      jk.rearrange("p (h d) -> p h d", d=96), bjk, sk, bsk)
            P.copy("act", vb.rearrange("p (h d) -> p h d", d=64), kvf[:, :, 64:128], reads=[bkvf], writes=[bvb])
            P.dma("pool", C.V[tok, :], vb, reads=[bvb], writes=[C.b_V])
        tsl = slice(sub * 128, (sub + 1) * 128)
        transpose_to(C, qn, bqn, NBQ, 5, qT[:, :, tsl], bqT, blkw=BW, evac="act")
        transpose_to(C, kn, bkn, NBQ, 6, kT[:, :, tsl], bkT, blkw=BW, evac="dve")
        transpose_to(C, qmn, bqmn, 2, 7, qmT[:, :, tsl], bqmT, evac="act")
        sm, bsm = small.next()
        if kind == 0:
            P.tt("dve", sm[:, 0, 0:NH], psb_[:, 2304:2316], bfb, ALU.add, reads=[bp, b_bfb], writes=[bsm])
            P.act(sm[:, 1, 0:NH], sm[:, 0, 0:NH], AF.Exp, scale=-1.0, reads=[bsm], writes=[bsm])
            P.ts("dve", sm[:, 2, 0:NH], sm[:, 1, 0:NH], 1.0, None, ALU.add, reads=[bsm], writes=[bsm])
            P.act(sm[:, 3, 0:NH], sm[:, 2, 0:NH], AF.Ln, reads=[bsm], writes=[bsm])
            ps, bps = C.PS[7]
            P.mm(ps[:, 0:NH], C.tri, sm[:, 3, 0:NH], True, False, reads=[C.bK, bsm], writes=[bps])
            P.mm(ps[:, 0:NH], C.ones_f[0:1, :], carry, False, True, reads=[C.bK, b_carry], writes=[bps])
            P.mm(ps[0:1, 16:16 + NH], C.ones_f[:, 0:1], sm[:, 3, 0:NH], True, False, reads=[C.bK, bsm], writes=[bps])
            P.mm(ps[0:1, 16:16 + NH], C.ones_f[0:1, 0:1], carry, False, True, reads=[C.bK, b_carry], writes=[bps])
            P.copy("dve", C.bias_tab[:, tt, :], ps[:, 0:NH], reads=[bps], writes=[C.b_bias])
            P.copy("dve", carry, ps[0:1, 16:16 + NH], reads=[bps], writes=[b_carry])
        if kind != 1:
            pc, bpc = pcs.next()
            pc3 = pc.rearrange("p (h r) -> p h r", r=3)
            P.ts("dve", sm[:, 4, 0:NH], C.bias_tab[:, tt, :], -1.0, None, ALU.mult, reads=[C.b_bias], writes=[bsm])
            P.copy("dve", pc3[:, :, 0], sm[:, 4, 0:NH], reads=[bsm], writes=[bpc])
            P.tt("dve", sm[:, 5, 0:NH], sm[:, 4, 0:NH], pc3[:, :, 0], ALU.subtract, reads=[bsm, bpc], writes=[bsm])
            P.copy("dve", pc3[:, :, 1], sm[:, 5, 0:NH], reads=[bsm], writes=[bpc])
            P.tt("dve", sm[:, 6, 0:NH], sm[:, 5, 0:NH], pc3[:, :, 1], ALU.subtract, reads=[bsm, bpc], writes=[bsm])
            P.copy("dve", pc3[:, :, 2], sm[:, 6, 0:NH], reads=[bsm], writes=[bpc])
            ps, bps = C.PS[7]
            psb = ps.bitcast(BF16)
            P.transpose(psb[0:36, 256:384], pc, C.ident, reads=[bpc, C.bK], writes=[bps])
            P.copy("act", prow[:, tok], psb[0:36, 256:384], reads=[bps], writes=[b_prow])
        if kind == 2:
            ps, bps = C.PS[7]
            for pj in range(6):
                P.mm(ps[:, 256 + pj * 16:256 + (pj + 1) * 16], kn[:, pj * 128:(pj + 1) * 128], C.ones_bf[:, 0:16], True, True,
                     reads=[bkn, C.bK], writes=[bps])
            P.copy("dve", ksum[:, :, tt], ps[:, 256:352].rearrange("p (a b) -> p a b", b=16)[:, :, 0], reads=[bps], writes=[b_ksum])
            nvalid = tt // 2
            gate, bgate = gater.next()
            t8, bt8 = top8.next()
            nm, bnm = nmr.next()
            nm3 = nm.rearrange("p (h n) -> p h n", n=16)
            P.memset("dve", gate, -1e30, writes=[bgate])
            if nvalid > 0:
                qc, bqc = qtc.next()
                P.copy("pool", qc, qT[:, :, tsl], reads=[bqT], writes=[bqc])
                for pj in range(6):
                    P.mm(ps[:, 64 + pj * 32:64 + (pj + 1) * 32], qc[:, pj, :], kmean[:, pj, :], True, True,
                         reads=[bqc, b_kmean], writes=[bps])
                P.copy("dve", gate[:, :, 0:nvalid],
                       ps[:, 64:64 + NH * 16].rearrange("p (h n) -> p h n", n=16)[:, :, 0:nvalid],
                       reads=[bps], writes=[bgate])
            for h in range(NH):
                P.max8(t8[:, h, :], gate[:, h, :], reads=[bgate], writes=[bt8])
            P.tt("dve", gate, gate, bc_last(t8[:, :, 2], 16), ALU.subtract, reads=[bgate, bt8], writes=[bgate])
            P.ts("dve", gate, gate, 0.0, None, ALU.is_ge, reads=[bgate], writes=[bgate])
            P.ts("dve", nm3, gate, -NEG, NEG, ALU.mult, ALU.add, reads=[bgate], writes=[bnm])
            P.memset("dve", nm3[:, :, nvalid:nvalid + 1], 0.0, writes=[bnm])
            if nvalid + 1 < 16:
                P.memset("dve", nm3[:, :, nvalid + 1:16], NEG, writes=[bnm])
            psb = ps.bitcast(BF16)
            P.transpose(psb[:, 512:640], nm[:, 0:128], C.ident, reads=[bnm, C.bK], writes=[bps])
            P.transpose(psb[0:64, 640:768], nm[:, 128:192], C.ident, reads=[bnm, C.bK], writes=[bps])
            P.copy("act", nmrows[0][:, tok], psb[:, 512:640], reads=[bps], writes=[b_nmrows])
            P.copy("act", nmrows[1][:, tok], psb[0:64, 640:768], reads=[bps], writes=[b_nmrows])
            if tt % 2 == 1:
                n = tt // 2
                P.tt("dve", ksum[:, :, tt], ksum[:, :, tt], ksum[:, :, tt - 1], ALU.add, reads=[b_ksum], writes=[b_ksum])
                P.ts("dve", kmean[0:64, :, n], ksum[0:64, :, tt], 1.0 / 256, None, ALU.mult, reads=[b_ksum], writes=[b_kmean])
                P.ts("dve", kmean[64:128, :, 16 + n], ksum[64:128, :, tt], 1.0 / 256, None, ALU.mult, reads=[b_ksum], writes=[b_kmean])
        if sub == 3:
            c4 = slice((tt - 3) * 128, (tt + 1) * 128)
            if kind != 1:
                P.dma("pool", C.QT[0:768, c4].rearrange("(b p) s -> p b s", p=128), qT, reads=[bqT], writes=[C.b_QT])
                P.dma("pool", C.KT[0:768, c4].rearrange("(b p) s -> p b s", p=128), kT, reads=[bkT], writes=[C.b_KT])
            else:
                P.dma("pool", C.QT[:, c4].rearrange("(b p) s -> p b s", p=96), qT[0:96], reads=[bqT], writes=[C.b_QT])
                P.dma("pool", C.KT[:, c4].rearrange("(b p) s -> p b s", p=96), kT[0:96], reads=[bkT], writes=[C.b_KT])
            P.dma("pool", C.QmT[:, c4].rearrange("(b p) s -> p b s", p=128), qmT, reads=[bqmT], writes=[C.b_QmT])
    if kind != 1:
        for h in range(NH):
            P.dma("pool", C.AUX[h, 0:3, :], prow[3 * h:3 * h + 3, :], reads=[b_prow], writes=[C.b_AUX])
    if kind == 2:
        for h in range(NH):
            src = nmrows[0][16 * h:16 * h + 16, :] if h < 8 else nmrows[1][16 * (h - 8):16 * (h - 8) + 16, :]
            P.dma("pool", C.AUX[h, 3:19, :], src, reads=[b_nmrows], writes=[C.b_AUX])

def phaseB(C, kind):
    P, NT, S = C.P, C.NT, C.S
    NCH = S // 512
    DK = 96 if kind == 1 else 64
    R = {0: 3, 1: 0, 2: 19}[kind]
    qr = Ring(P, "b_q", 2, [96, S], BF16)
    kr = Ring(P, "b_k", 2, [96, S], BF16)
    vr = Ring(P, "b_v", 2, [128, NT, 128], BF16)
    ptr = Ring(P, "b_pt", 4, [128, 512], BF16)
    rdr = Ring(P, "b_rd", 2, [64, 512], F32)
    onr = Ring(P, "b_on", 2, [64, 512], BF16)
    for (v, bv) in vr.items:
        P.memset("pool", v[:, :, 64:128], 1.0, writes=[bv])
    st_banks = [0, 1, 2, 5]
    acc_banks = [3, 4]
    state = dict(si=0, ci=0)

    def load_head(h):
        q, bq = qr.next()
        if h < NH:
            k, bk = kr.next()
            v, bv = vr.next()
            P.dma("sp", q[0:DK, :], C.QT[h * DK:(h + 1) * DK, :], reads=[C.b_QT], writes=[bq])
            P.dma("sp", k[0:DK, :], C.KT[h * DK:(h + 1) * DK, :], reads=[C.b_KT], writes=[bk])
            P.dma("sp", v[:, :, 0:64], C.V[:, h * 64:(h + 1) * 64].rearrange("(t p) d -> p t d", p=128),
                  reads=[C.b_V], writes=[bv])
            if R:
                P.dma("sp", q[64:64 + R, :], C.AUX[h, 0:R, :], reads=[C.b_AUX], writes=[bq])
                P.dma("sp", k[64:64 + R, :], C.I["c_selS"][0:R, :], writes=[bk])
            return dict(q=q, bq=bq, k=k, bk=bk, v=v, bv=bv)
        hm = h - NH
        P.dma("sp", q[0:64, :], C.QmT[hm * 64:(hm + 1) * 64, :], reads=[C.b_QmT], writes=[bq])
        return dict(q=q, bq=bq)

    def run_head(h, L):
        mem = h >= NH
        dk = 64 if mem else DK + R
        steps = []
        for c in range(NCH):
            nk = 2 if mem else 4 * c + 4
            for kt in range(nk):
                steps.append((c, kt, kt == 0, kt == nk - 1))
        LOOK = 3
        pend = []
        accs = {}
        for i in range(len(steps) + LOOK):
            if i < len(steps):
                c, kt, first, last = steps[i]
                jd = -1 if mem else kt - 4 * c
                c0 = 128 * jd if jd > 0 else 0
                qs = L["q"][0:dk, c * 512 + c0:(c + 1) * 512]
                ps, bps = C.PS[st_banks[state["si"] % 4]]
                state["si"] += 1
                pt, bpt = ptr.next()
                if mem:
                    ksl = C.kmT[:, h - NH, kt * 128:(kt + 1) * 128]
                    P.mm(ps[:, c0:512], ksl, qs, True, True, reads=[C.b_kmT, L["bq"]], writes=[bps])
                    P.act(pt[:, c0:512], ps[:, c0:512], AF.Exp, reads=[bps], writes=[bpt])
                else:
                    ksl = L["k"][0:dk, kt * 128:(kt + 1) * 128]
                    more = jd >= 0
                    P.mm(ps[:, c0:512], ksl, qs, True, not more, reads=[L["bk"], L["bq"]], writes=[bps])
                    if jd >= 0:
                        P.mm(ps[:, c0:c0 + 128], C.ident, C.maskneg, False, True, reads=[C.bK], writes=[bps])
                    if R:
                        P.act(pt[:, c0:512], ps[:, c0:512], AF.Exp, bias=C.bias_tab[:, kt, h:h + 1],
                              reads=[bps, C.b_bias], writes=[bpt])
                    else:
                        P.act(pt[:, c0:512], ps[:, c0:512], AF.Exp, reads=[bps], writes=[bpt])
                pend.append((c, kt, first, last, c0, pt, bpt))
            if i >= LOOK:
                c, kt, first, last, c0, pt, bpt = pend.pop(0)
                if first:
                    accs[c] = C.PS[acc_banks[state["ci"] % 2]]
                    state["ci"] += 1
                acc, bacc = accs[c]
                if mem:
                    vsl, bvs = C.vmaug[:, kt, h - NH, :], C.b_vmaug
                else:
                    vsl, bvs = L["v"][:, kt, :], L["bv"]
                P.mm(acc[:, c0:512], vsl, pt[:, c0:512], first, last, reads=[bvs, bpt], writes=[bacc])
                if last:
                    rd, brd = rdr.next()
                    on, bon = onr.next()
                    P.recip(rd, acc[64:128, :], reads=[bacc], writes=[brd])
                    P.tt("dve", on, acc[0:64, :], rd, ALU.mult, reads=[bacc, brd], writes=[bon])
                    P.dma("pool", C.OT[h * 64:(h + 1) * 64, c * 512:(c + 1) * 512], on, reads=[bon], writes=[C.b_OT])
                    del accs[c]

    nxt = load_head(0)
    for h in range(NH + NMH):
        cur = nxt
        if h + 1 < NH + NMH:
            nxt = load_head(h + 1)
        run_head(h, cur)

def phaseC(C, li, xsrc):
    P, I, NT = C.P, C.I, C.NT
    wo = P.sbuf("w_out", [128, 8, D], BF16)
    b_wo = Buf("w_out")
    P.dma("pool", wo, I["w_out"][li].rearrange("(c p) n -> p c n", p=128), writes=[b_wo])
    otr = Ring(P, "c_ot", 2, [128, 8, 512], BF16)
    xr = Ring(P, "c_x", 3, [128, D], F32)
    banks = [0, 1, 2, 3]
    bi = 0
    for tt in range(NT):
        tok = slice(tt * 128, (tt + 1) * 128)
        if tt % 4 == 0:
            ot, bot = otr.next()
            P.dma("sp", ot, C.OT[:, tt * 128:(tt + 4) * 128].rearrange("(c p) s -> p c s", p=128),
                  reads=[C.b_OT], writes=[bot])
        xt, bx = xr.next()
        P.dma("sp", xt, xsrc[tok, :], reads=[C.b_y[tt]], writes=[bx])
        sub = tt % 4
        for g in range(2):
            ps, bps = C.PS[banks[bi % 4]]
            bi += 1
            for k in range(8):
                P.mm(ps, ot[:, k, sub * 128:(sub + 1) * 128], wo[:, k, g * 512:(g + 1) * 512], k == 0, k == 7,
                     reads=[bot, b_wo], writes=[bps])
            P.tt("dve", xt[:, g * 512:(g + 1) * 512], xt[:, g * 512:(g + 1) * 512], ps, ALU.add,
                 reads=[bx, bps], writes=[bx])
        P.dma("pool", C.y[tok, :], xt, reads=[bx], writes=[C.b_y[tt]])


def phaseD(C, li):
    P, I, NT, S = C.P, C.I, C.NT, C.S
    CH = 256
    NCH = S // CH
    wu = P.sbuf("w_up", [128, 8, 2 * DFF], BF16)
    b_wu = Buf("w_up")
    for k in range(8):
        P.dma("pool", wu[:, k, :], I["ffn_w_up"][li][k * 128:(k + 1) * 128, :], writes=[b_wu])
    wd = P.sbuf("w_dn", [128, NJ, D], BF16)
    b_wd = Buf("w_dn")
    P.dma("pool", wd, I["ffn_w_down"][li].rearrange("(c p) n -> p c n", p=128), writes=[b_wd])
    gf, b_gf = load_gain(C, "g_ffn", I["norm_ffn"][li], D)
    craw = P.sbuf("craw", [44, 4, 128], F32)
    b_craw = Buf("craw")
    for w in range(3):
        P.dma("sp", craw[:, w, :], I["ffn_conv_w"][li, w].rearrange("(c p) -> c p", p=128), writes=[b_craw])
    P.dma("sp", craw[:, 3, :], I["ffn_conv_b"][li].rearrange("(c p) -> c p", p=128), writes=[b_craw])
    cw = P.sbuf("cw", [128, 4, 44], F32)
    b_cw = Buf("cw")
    ps, bps = C.PS[7]
    for w in range(4):
        P.mm(ps[:, w * 64:w * 64 + 44], craw[:, w, :], C.identf[0:44, 0:44], True, True, reads=[b_craw, C.bK], writes=[bps])
    P.copy("dve", cw, ps[:, 0:256].rearrange("p (w c) -> p w c", c=64)[:, :, 0:44], reads=[bps], writes=[b_cw])
    car = P.sbuf("car", [128, 44, 2], F32)
    b_car = Buf("car")
    P.memset("dve", car, 0.0, writes=[b_car])
    xr = Ring(P, "d_x", 3, [128, D], F32)
    jr = Ring(P, "d_junk", 1, [128, D], F32)
    sr = Ring(P, "d_st", 2, [128, 4], F32)
    hr = Ring(P, "d_h", 2, [128, D], BF16)
    h2r = Ring(P, "d_h2T", 1, [128, 8, CH], BF16)
    atr = Ring(P, "d_at", 1, [128, NJ, CH], BF16)
    ugr = Ring(P, "d_ug", 2, [128, CH + 2], F32)
    uvr = Ring(P, "d_uv", 2, [128, CH + 2], F32)
    cgr = Ring(P, "d_cg", 2, [128, 2, CH], F32)
    cvr = Ring(P, "d_cv", 2, [128, 2, CH], F32)
    gb_banks = [1, 2]
    vb_banks = [3, 4]
    dn_banks = [5, 6]
    di = 0
    for ch in range(NCH):
        h2T, bh2 = h2r.next()
        for s2 in range(CH // 128):
            tt = ch * (CH // 128) + s2
            tok = slice(tt * 128, (tt + 1) * 128)
            xt, bx = xr.next()
            junk, bj = jr.next()
            st, bst = sr.next()
            hb, bh = hr.next()
            P.dma("sp", xt, C.y[tok, :], reads=[C.b_y[tt]], writes=[bx])
            rms_rows(C, xt, bx, D, gf, b_gf, hb, bh, junk, bj, st, bst)
            transpose_to(C, hb, bh, 8, 0, h2T[:, :, s2 * 128:(s2 + 1) * 128], bh2)
        at, bat = atr.next()
        for j in range(NJ):
            pg, bpg = C.PS[gb_banks[j % 2]]
            pv, bpv = C.PS[vb_banks[j % 2]]
            for k in range(8):
                P.mm(pg[:, 0:CH], wu[:, k, j * 128:(j + 1) * 128], h2T[:, k, :], k == 0, k == 7,
                     reads=[b_wu, bh2], writes=[bpg])
            for k in range(8):
                P.mm(pv[:, 0:CH], wu[:, k, DFF + j * 128:DFF + (j + 1) * 128], h2T[:, k, :], k == 0, k == 7,
                     reads=[b_wu, bh2], writes=[bpv])
            res = []
            for (pp, bpp, ring, cring, jj) in ((pg, bpg, ugr, cgr, j), (pv, bpv, uvr, cvr, NJ + j)):
                u, bu = ring.next()
                cc, bcc = cring.next()
                P.copy("pool", u[:, 0:2], car[:, jj, :], reads=[b_car], writes=[bu])
                P.copy("act", u[:, 2:CH + 2], pp[:, 0:CH], reads=[bpp], writes=[bu])
                P.copy("pool", car[:, jj, :], u[:, CH:CH + 2], reads=[bu], writes=[b_car])
                P.act(cc[:, 0, :], u[:, 2:CH + 2], AF.Identity, scale=cw[:, 2, jj:jj + 1], bias=cw[:, 3, jj:jj + 1],
                      reads=[bu, b_cw], writes=[bcc])
                P.stt(cc[:, 1, :], u[:, 1:CH + 1], cw[:, 1, jj:jj + 1], cc[:, 0, :], ALU.mult, ALU.add,
                      reads=[bu, b_cw, bcc], writes=[bcc])
                P.stt(cc[:, 0, :], u[:, 0:CH], cw[:, 0, jj:jj + 1], cc[:, 1, :], ALU.mult, ALU.add,
                      reads=[bu, b_cw, bcc], writes=[bcc])
                res.append((cc, bcc))
            (cg, bcg), (cv, bcv) = res
            P.act(cg[:, 1, :], cg[:, 0, :], AF.Silu, reads=[bcg], writes=[bcg])
            P.tt("dve", at[:, j, :], cg[:, 1, :], cv[:, 0, :], ALU.mult, reads=[bcg, bcv], writes=[bat])
        for s2 in range(CH // 128):
            tt = ch * (CH // 128) + s2
            tok = slice(tt * 128, (tt + 1) * 128)
            xt, bx = xr.next()
            ot, bo = xt, bx
            P.dma("sp", xt, C.y[tok, :], reads=[C.b_y[tt]], writes=[bx])
            for g in range(2):
                pd, bpd = C.PS[dn_banks[di % 2]]
                di += 1
                for j in range(NJ):
                    P.mm(pd, at[:, j, s2 * 128:(s2 + 1) * 128], wd[:, j, g * 512:(g + 1) * 512], j == 0, j == NJ - 1,
                         reads=[bat, b_wd], writes=[bpd])
                P.tt("dve", ot[:, g * 512:(g + 1) * 512], xt[:, g * 512:(g + 1) * 512], pd, ALU.add,
                     reads=[bx, bpd], writes=[bo])
            P.dma("pool", C.y[tok, :], ot, reads=[bo], writes=[C.b_y[tt]])


def host_consts(S):
    import ml_dtypes
    bf = ml_dtypes.bfloat16
    idx = np.arange(128)
    c = {}
    c["c_ident"] = np.eye(128, dtype=np.float32).astype(bf)
    c["c_identf"] = np.eye(128, dtype=np.float32)
    c["c_tri"] = (idx[:, None] <= idx[None, :]).astype(np.float32)
    c["c_maskneg"] = np.where(idx[:, None] > idx[None, :], NEG, 0.0).astype(np.float32).astype(bf)
    sel = np.zeros((19, 16, 128), np.float32)
    sel[0:3] = 1.0
    for n in range(16):
        sel[3 + n, n, :] = 1.0
    c["c_sel"] = sel.reshape(19, 16 * 128).astype(bf)
    selS = np.zeros((19, S), np.float32)
    selS[0:3] = 1.0
    blk = np.arange(S) // 256
    for n in range(16):
        selS[3 + n, blk == n] = 1.0
    c["c_selS"] = selS.astype(bf)
    c["c_invfreq"] = (np.float32(10000.0) ** (-(np.arange(0, 32, 2, dtype=np.float32)) / np.float32(32))).astype(np.float32)
    c["c_slopes"] = (2.0 ** (-8.0 * np.arange(1, NH + 1, dtype=np.float32) / NH)).astype(np.float32)
    return c


_WEIGHTS = ["norm_mix", "norm_ffn", "w_out", "mem_norm", "w_mem_kv", "mem_q_gain", "mem_k_gain", "fox_w_in",
            "fox_b_f", "fox_q_gain", "fox_k_gain", "mla_w_in", "mla_qa_norm", "mla_kva_norm", "mla_w_q_up",
            "mla_w_kv_up", "mla_q_gain", "mla_k_gain", "moba_w_in", "moba_q_gain", "moba_k_gain", "ffn_w_up",
            "ffn_conv_w", "ffn_conv_b", "ffn_w_down"]


def make_in_maps(inputs, cores, S):
    consts = host_consts(S)
    shared = {k: np.ascontiguousarray(np.asarray(inputs[k])) for k in _WEIGHTS}
    maps = []
    for b in cores:
        m = dict(shared)
        m.update(consts)
        m["x"] = np.ascontiguousarray(np.asarray(inputs["x"])[b])
        m["mem"] = np.ascontiguousarray(np.asarray(inputs["mem"])[b])
        m["positions"] = np.ascontiguousarray(np.asarray(inputs["positions"])[b].reshape(S, 1).astype(np.int32))
        maps.append(m)
    return maps


def kernel(**inputs):
    B, S, _ = inputs["x"].shape
    nc = build_program(S, [0, 1, 2, 3])
    maps = make_in_maps(inputs, list(range(B)), S)
    res = run_bass_kernel_spmd(nc, maps, core_ids=list(range(B)))
    return np.stack([np.asarray(r["y"]) for r in res.results], axis=0).astype(np.float32)
```
